# Optimizing a Trainium2 kernel written in Bass

```python
import jax, jax.numpy as jnp
from jax import lax
import numpy as np

D_MODEL = 1024
BATCH = 8
SEQ = 4096
DEPTH = 4

D_MIX = D_MODEL
SB_HEAD_DIM = 64
SB_HEADS = (D_MIX // 2) // SB_HEAD_DIM
SB_WIDTH = SB_HEADS * SB_HEAD_DIM
SB_BLOCK = 128
GLA_HEADS = 4
GLA_DV = (D_MIX // 2) // GLA_HEADS
GLA_DK = GLA_DV // 2
GLA_K_WIDTH = GLA_HEADS * GLA_DK
GLA_V_WIDTH = GLA_HEADS * GLA_DV
GLA_RANK = 16
GLA_GATE_NORM = 16.0
GLA_CHUNK = 64
D_IN_PROJ = 3 * SB_WIDTH + 2 * GLA_K_WIDTH + 2 * GLA_V_WIDTH + GLA_RANK
N_GROUPS = 4
EXPERTS_PER_GROUP = 8
TOP_K_IN_GROUP = 2
D_EXPERT = 256
N_MOD = 6
EPS = 1e-6

kernel_name = "hybrid_sb_gla_hmoe_adaln"


def rms_norm(x, gain):
    xf = x.astype(jnp.float32)
    y = xf * lax.rsqrt(jnp.mean(xf * xf, axis=-1, keepdims=True) + EPS)
    return (y * gain.astype(jnp.float32)).astype(x.dtype)


def split_heads(t, n_heads):
    b, s, w = t.shape
    return t.reshape(b, s, n_heads, w // n_heads).transpose(0, 2, 1, 3)


def merge_heads(t):
    b, h, s, d = t.shape
    return t.transpose(0, 2, 1, 3).reshape(b, s, h * d)


def stick_breaking_attention(q, k, v):
    B, H, S, d = q.shape
    nb = S // SB_BLOCK
    scale = d ** -0.5
    q_blocks = jnp.moveaxis(q.reshape(B, H, nb, SB_BLOCK, d), 2, 0)
    key_pos = jnp.arange(S)

    def one_block(args):
        q_blk, blk = args
        z = jnp.einsum('bhqd,bhkd->bhqk', q_blk, k).astype(jnp.float32) * scale
        query_pos = blk * SB_BLOCK + jnp.arange(SB_BLOCK)
        before = key_pos[None, :] < query_pos[:, None]
        log_fail = jnp.where(before, -jax.nn.softplus(z), 0.0)
        suffix = lax.cumsum(log_fail, axis=3, reverse=True) - log_fail
        w = jnp.where(before, jnp.exp(jax.nn.log_sigmoid(z) + suffix), 0.0)
        return jnp.einsum('bhqk,bhkd->bhqd', w.astype(v.dtype), v)

    out = lax.map(one_block, (q_blocks, jnp.arange(nb)))
    return jnp.moveaxis(out, 0, 2).reshape(B, H, S, d)


def gla_chunked(q, k, v, log_g):
    B, H, S, dk = q.shape
    dv = v.shape[-1]
    C = GLA_CHUNK
    nc = S // C
    f32 = jnp.float32
    qc = q.astype(f32).reshape(B, H, nc, C, dk) * (dk ** -0.5)
    kc = k.astype(f32).reshape(B, H, nc, C, dk)
    vc = v.astype(f32).reshape(B, H, nc, C, dv)
    b = jnp.cumsum(log_g.astype(f32).reshape(B, H, nc, C, dk), axis=3)
    b_last = b[:, :, :, -1:, :]
    q_dec = qc * jnp.exp(b)
    k_inv = kc * jnp.exp(-b)
    causal = jnp.tril(jnp.ones((C, C), dtype=bool))
    scores = jnp.where(causal, jnp.einsum('bhnqd,bhnkd->bhnqk', q_dec, k_inv), 0.0)
    intra = jnp.einsum('bhnqk,bhnkv->bhnqv', scores, vc)
    chunk_kv = jnp.einsum('bhnkd,bhnkv->bhndv', kc * jnp.exp(b_last - b), vc)
    chunk_decay = jnp.exp(b_last[:, :, :, 0, :])

    def step(state, inp):
        dec, kv_c = inp
        return dec[..., None] * state + kv_c, state

    _, prev = lax.scan(step, jnp.zeros((B, H, dk, dv), f32),
                       (jnp.moveaxis(chunk_decay, 2, 0), jnp.moveaxis(chunk_kv, 2, 0)))
    prev = jnp.moveaxis(prev, 0, 2)
    inter = jnp.einsum('bhnqd,bhndv->bhnqv', q_dec, prev)
    return (intra + inter).reshape(B, H, S, dv).astype(v.dtype)


def hybrid_mixer(h, w_in, w_gk2, b_gk, q_gain, k_gain, gla_gain, w_out):
    proj = h @ w_in
    widths = (SB_WIDTH, SB_WIDTH, SB_WIDTH, GLA_K_WIDTH, GLA_K_WIDTH,
              GLA_V_WIDTH, GLA_V_WIDTH, GLA_RANK)
    cuts = np.cumsum(widths)[:-1].tolist()
    sb_q, sb_k, sb_v, g_q, g_k, g_v, g_r, g_lr = jnp.split(proj, cuts, axis=-1)
    qa = rms_norm(split_heads(sb_q, SB_HEADS), q_gain)
    ka = rms_norm(split_heads(sb_k, SB_HEADS), k_gain)
    out_a = merge_heads(stick_breaking_attention(qa, ka, split_heads(sb_v, SB_HEADS)))
    log_g = jax.nn.log_sigmoid((g_lr @ w_gk2 + b_gk).astype(jnp.float32)) / GLA_GATE_NORM
    o_b = gla_chunked(split_heads(g_q, GLA_HEADS), split_heads(g_k, GLA_HEADS),
                      split_heads(g_v, GLA_HEADS), split_heads(log_g, GLA_HEADS))
    out_b = merge_heads(rms_norm(o_b, gla_gain)) * jax.nn.silu(g_r)
    return jnp.concatenate([out_a, out_b], axis=-1) @ w_out


def hierarchical_moe(h, w_router_grp, b_router_grp, w_router_exp, b_router_exp,
                     w_gate, w_up, w_down):
    B, S, D = h.shape
    hf = h.reshape(B * S, D)
    f32 = jnp.float32
    grp_prob = jax.nn.softmax((hf @ w_router_grp + b_router_grp).astype(f32), axis=-1)
    grp_w, grp_idx = lax.top_k(grp_prob, 1)
    exp_logits = (hf @ w_router_exp + b_router_exp).astype(f32).reshape(-1, N_GROUPS, EXPERTS_PER_GROUP)
    sel_logits = jnp.take_along_axis(exp_logits, grp_idx[:, :, None], axis=1)[:, 0]
    top_logit, top_idx = lax.top_k(sel_logits, TOP_K_IN_GROUP)
    top_w = jax.nn.softmax(top_logit, axis=-1) * grp_w
    exp_w = jnp.sum(jax.nn.one_hot(top_idx, EXPERTS_PER_GROUP, dtype=f32) * top_w[..., None], axis=1)
    comb = (jax.nn.one_hot(grp_idx[:, 0], N_GROUPS, dtype=f32)[:, :, None]
            * exp_w[:, None, :]).astype(h.dtype)
    y = jnp.zeros_like(hf)
    for g in range(N_GROUPS):
        a = jnp.einsum('nd,edf->nef', hf, w_gate[g])
        u = jnp.einsum('nd,edf->nef', hf, w_up[g])
        y = y + jnp.einsum('nef,efd->nd', jax.nn.silu(a) * u * comb[:, g, :, None], w_down[g])
    return y.reshape(B, S, D)


def setup_inputs(seed: int = 0) -> dict:
    key = jax.random.key(seed)
    ks = jax.random.split(key, 20)
    f32 = jnp.float32
    nrm = lambda k, shape, s: jax.random.normal(k, shape, f32) * s
    L, D = DEPTH, D_MODEL
    return {
        "x": nrm(ks[0], (BATCH, SEQ, D), 1.0),
        "c": nrm(ks[1], (BATCH, D), 1.0),
        "w_ada": nrm(ks[2], (L, D, N_MOD * D), 0.5 * D ** -0.5),
        "b_ada": nrm(ks[3], (L, N_MOD * D), 0.02),
        "norm_mix": 1.0 + nrm(ks[4], (L, D), 0.02),
        "norm_ffn": 1.0 + nrm(ks[5], (L, D), 0.02),
        "w_in": nrm(ks[6], (L, D, D_IN_PROJ), D ** -0.5),
        "w_gk2": nrm(ks[7], (L, GLA_RANK, GLA_K_WIDTH), GLA_RANK ** -0.5),
        "b_gk": 2.0 + nrm(ks[8], (L, GLA_K_WIDTH), 0.1),
        "q_gain": 1.0 + nrm(ks[9], (L, SB_HEAD_DIM), 0.02),
        "k_gain": 1.0 + nrm(ks[10], (L, SB_HEAD_DIM), 0.02),
        "gla_gain": 1.0 + nrm(ks[11], (L, GLA_DV), 0.02),
        "w_out": nrm(ks[12], (L, D_MIX, D), D_MIX ** -0.5),
        "w_router_grp": nrm(ks[13], (L, D, N_GROUPS), D ** -0.5),
        "b_router_grp": nrm(ks[14], (L, N_GROUPS), 0.01),
        "w_router_exp": nrm(ks[15], (L, D, N_GROUPS * EXPERTS_PER_GROUP), D ** -0.5),
        "b_router_exp": nrm(ks[16], (L, N_GROUPS * EXPERTS_PER_GROUP), 0.01),
        "w_gate": nrm(ks[17], (L, N_GROUPS, EXPERTS_PER_GROUP, D, D_EXPERT), D ** -0.5),
        "w_up": nrm(ks[18], (L, N_GROUPS, EXPERTS_PER_GROUP, D, D_EXPERT), D ** -0.5),
        "w_down": nrm(ks[19], (L, N_GROUPS, EXPERTS_PER_GROUP, D_EXPERT, D), D_EXPERT ** -0.5),
    }


def reference(x, c, w_ada, b_ada, norm_mix, norm_ffn, w_in, w_gk2, b_gk, q_gain, k_gain,
              gla_gain, w_out, w_router_grp, b_router_grp, w_router_exp, b_router_exp,
              w_gate, w_up, w_down):
    c_act = jax.nn.silu(c)
    for l in range(DEPTH):
        mod = (c_act @ w_ada[l] + b_ada[l])[:, None, :]
        shift_m, scale_m, gate_m, shift_f, scale_f, gate_f = jnp.split(mod, N_MOD, axis=-1)
        h = rms_norm(x, norm_mix[l]) * (1.0 + scale_m) + shift_m
        x = x + gate_m * hybrid_mixer(h, w_in[l], w_gk2[l], b_gk[l], q_gain[l], k_gain[l],
                                      gla_gain[l], w_out[l])
        h = rms_norm(x, norm_ffn[l]) * (1.0 + scale_f) + shift_f
        x = x + gate_f * hierarchical_moe(h, w_router_grp[l], b_router_grp[l], w_router_exp[l],
                                          b_router_exp[l], w_gate[l], w_up[l], w_down[l])
    return x
```

```python
import numpy as np
from contextlib import ExitStack
import concourse.bass as bass
import concourse.mybir as mybir
from concourse.bass_utils import run_bass_kernel_spmd

F32 = mybir.dt.float32
BF16 = mybir.dt.bfloat16
AF = mybir.ActivationFunctionType
ALU = mybir.AluOpType
AX = mybir.AxisListType

D = 1024
KC = 8
DIN = 3088
NMOD = 6 * D
EPS = 1e-6
BIG = 1.0e30
TG = 512

ENGS = ("pe", "act", "dve", "pool", "sp")
TICK_LIMIT = 30000


class Op:
    __slots__ = ("eng", "fn", "deps", "dma", "needed", "tick", "dsem", "dval")

    def __init__(self, eng, fn, dma):
        self.eng = eng
        self.fn = fn
        self.deps = []
        self.dma = dma
        self.needed = False
        self.tick = None
        self.dsem = None
        self.dval = None


class Prog:
    def __init__(self, nc, n_dma_sems=40, sems_per_eng=12):
        self.nc = nc
        self.ops = {e: [] for e in ENGS}
        self.last_w = {}
        self.readers = {}
        self.n_dma_sems = n_dma_sems
        self.sems_per_eng = sems_per_eng
        self.n_sw = 10
        self.n_hw = n_dma_sems - self.n_sw
        self.dma_count = 0
        self.sw_count = 0
        self.recent_dmas = []
        self.recent_sw = []

    def add(self, eng, fn, reads=(), writes=(), dma=False):
        op = Op(eng, fn, dma)
        deps = set()
        for r in reads:
            w = self.last_w.get(r)
            if w is not None:
                deps.add(w)
        for w_ in writes:
            w = self.last_w.get(w_)
            if w is not None:
                deps.add(w)
            for rd in self.readers.get(w_, ()):
                deps.add(rd)
        for r in reads:
            self.readers.setdefault(r, []).append(op)
        for w_ in writes:
            self.last_w[w_] = op
            self.readers[w_] = []
        deps.discard(op)
        for d in deps:
            if (not d.dma) and d.eng == "pe" and eng == "pe" and not dma:
                continue
            op.deps.append(d)
            d.needed = True
        if dma:
            if eng == "pool":
                op.dsem = self.n_hw + self.sw_count % self.n_sw
                op.dval = 16 * (self.sw_count // self.n_sw + 1)
                self.sw_count += 1
                self.recent_sw.append(op)
                if len(self.recent_sw) > self.n_sw:
                    self.recent_sw.pop(0)
            else:
                op.dsem = self.dma_count % self.n_hw
                op.dval = 16 * (self.dma_count // self.n_hw + 1)
                self.dma_count += 1
                self.recent_dmas.append(op)
                if len(self.recent_dmas) > self.n_hw:
                    self.recent_dmas.pop(0)
        self.ops[eng].append(op)
        return op

    def barrier(self):
        marks = []
        for e in ENGS:
            for o in reversed(self.ops[e]):
                if not o.dma and o.fn is not None:
                    marks.append(o)
                    break
        marks = [m for m in marks if m.eng != "sp"]
        dmas = list(self.recent_dmas) + list(self.recent_sw)
        for e in ENGS:
            nop = Op(e, None, False)
            for d in marks + dmas:
                nop.deps.append(d)
                d.needed = True
            self.ops[e].append(nop)
        self.last_w = {}
        self.readers = {}

    def emit(self):
        nc = self.nc
        with ExitStack() as es:
            esems = {e: [es.enter_context(nc.semaphore(f"t_{e}_{i}")) for i in range(self.sems_per_eng)]
                     for e in ENGS if e != "sp"}
            dsems = [es.enter_context(nc.semaphore(f"d_{i}")) for i in range(self.n_dma_sems)]
            for e in ENGS:
                cnt = 0
                epoch = 0
                for op in self.ops[e]:
                    if op.dma or not op.needed:
                        continue
                    if e == "sp":
                        raise RuntimeError("sp compute op cannot be depended on")
                    cnt += 1
                    if cnt > TICK_LIMIT:
                        epoch += 1
                        cnt = 1
                    op.tick = (epoch, cnt)
            block = es.enter_context(nc.Block())
            engmap = {"pe": block.tensor, "act": block.scalar, "dve": block.vector,
                      "pool": block.gpsimd, "sp": block.sync}

            def make(e):
                def body(eng):
                    waited = {}

                    def wait(sem, key, val):
                        if waited.get(key, 0) >= val:
                            return
                        eng.wait_ge(sem, val)
                        waited[key] = val

                    for op in self.ops[e]:
                        for d in op.deps:
                            if d.dma:
                                wait(dsems[d.dsem], ("d", d.dsem), d.dval)
                            else:
                                ep, t = d.tick
                                wait(esems[d.eng][ep], (d.eng, ep), t)
                        if op.dma:
                            if op.dval > 16:
                                wait(dsems[op.dsem], ("d", op.dsem), op.dval - 16)
                            ins = op.fn(eng)
                            ins.then_inc(dsems[op.dsem], 16)
                        elif op.fn is not None:
                            ins = op.fn(eng)
                            if op.needed:
                                ep, t = op.tick
                                ins.then_inc(esems[e][ep], 1)
                    for op in self.ops[e]:
                        if op.dma:
                            wait(dsems[op.dsem], ("d", op.dsem), op.dval)
                return body

            for e in ENGS:
                engmap[e](make(e))


class SbAlloc:
    def __init__(self, nc, start=16640, limit=229376 - 256):
        self.nc = nc
        self.limit = limit
        self.base = start
        self.cur = start
        self.n = 0

    def _al(self, name, shape, dt, persistent):
        nbytes = int(np.prod(shape[1:])) * (4 if dt == F32 else 2)
        nbytes = (nbytes + 63) // 64 * 64
        off = self.cur
        if off + nbytes > self.limit:
            raise RuntimeError(f"SBUF overflow allocating {name}: {off}+{nbytes}")
        self.cur += nbytes
        if persistent:
            assert self.base == off, "persistent allocs must come first"
            self.base = self.cur
        self.n += 1
        return self.nc.alloc_sbuf_tensor_at(f"{name}_{self.n}", list(shape), dt, offset=off)

    def pers(self, name, shape, dt):
        return self._al(name, shape, dt, True)

    def ph(self, name, shape, dt):
        return self._al(name, shape, dt, False)

    def reset(self):
        self.cur = self.base


def build(S, DEPTH):
    NT = S // 128
    NG = S // TG
    NHALF = 2 if NG >= 2 else 1
    GH = NG // NHALF
    HS = GH * TG

    nc = bass.Bass("TRN2", target_bir_lowering=False)
    dram_in = lambda name, shape: nc.dram_tensor(name, list(shape), F32, kind="ExternalInput").ap()
    x_d = dram_in("x", [S, D])
    c_d = dram_in("c_col", [128, KC])
    wada_d = dram_in("w_ada", [DEPTH, D, NMOD])
    bada_d = dram_in("b_ada_col", [DEPTH, 128, 48])
    nm_d = dram_in("nm_col", [DEPTH, 128, KC])
    nf_d = dram_in("nf_col", [DEPTH, 128, KC])
    win_d = dram_in("w_in", [DEPTH, D, DIN])
    w2_d = dram_in("w2aug", [DEPTH, 32, 256])
    qg_d = dram_in("qg_col", [128, DEPTH])
    kg_d = dram_in("kg_col", [128, DEPTH])
    gg_d = dram_in("gg_col", [128, DEPTH])
    wout_d = dram_in("w_out", [DEPTH, D, D])
    wr_d = dram_in("w_r", [DEPTH, D, 36])
    br_d = dram_in("b_rT", [36, DEPTH])
    wg_d = dram_in("w_gate", [DEPTH, 32, D, 256])
    wu_d = dram_in("w_up", [DEPTH, 32, D, 256])
    wd_d = dram_in("w_down", [DEPTH, 32, 256, D])
    out_d = nc.dram_tensor("out", [S, D], F32, kind="ExternalOutput").ap()

    xT_d = nc.dram_tensor("xT_scr", [D, S], F32).ap()
    qT_d = nc.dram_tensor("qT_scr", [512, S], BF16).ap()
    kT_d = nc.dram_tensor("kT_scr", [512, S], BF16).ap()
    v_d = nc.dram_tensor("v_scr", [S, 512], BF16).ap()
    mixT_d = nc.dram_tensor("mixT_scr", [D, S], BF16).ap()

    xT_v = xT_d.rearrange("(k p) s -> p k s", p=128)
    mixT_v = mixT_d.rearrange("(k p) s -> p k s", p=128)

    P = Prog(nc)
    A = SbAlloc(nc)
    banks = [nc.alloc_psum_tensor(f"bank{i}", [128, 512], F32) for i in range(8)]

    def MM(out, lhsT, rhs, start, stop, r, w):
        P.add("pe", lambda e: e.matmul(out, lhsT=lhsT, rhs=rhs, start=start, stop=stop), r, w)

    def TR(out, in_, ident, r, w):
        P.add("pe", lambda e: e.transpose(out=out, in_=in_, identity=ident), r, w)

    def ACT(out, in_, func, r, w, bias=0.0, scale=1.0, accum_out=None):
        if accum_out is None:
            P.add("act", lambda e: e.activation(out=out, in_=in_, func=func, bias=bias, scale=scale), r, w)
        else:
            P.add("act", lambda e: e.activation(out=out, in_=in_, func=func, bias=bias, scale=scale,
                                                accum_out=accum_out), r, w)

    def STT(out, in0, scalar, in1, op0, op1, r, w):
        P.add("dve", lambda e: e.scalar_tensor_tensor(out=out, in0=in0, scalar=scalar, in1=in1,
                                                      op0=op0, op1=op1), r, w)

    def TS(out, in0, s1, s2, op0, op1, r, w, eng="dve"):
        if s2 is None:
            P.add(eng, lambda e: e.tensor_scalar(out=out, in0=in0, scalar1=s1, scalar2=None, op0=op0), r, w)
        else:
            P.add(eng, lambda e: e.tensor_scalar(out=out, in0=in0, scalar1=s1, scalar2=s2, op0=op0, op1=op1), r, w)

    def TT(out, in0, in1, op, r, w, eng="dve"):
        P.add(eng, lambda e: e.tensor_tensor(out=out, in0=in0, in1=in1, op=op), r, w)

    def CP(out, in_, r, w, eng="dve"):
        P.add(eng, lambda e: e.tensor_copy(out=out, in_=in_), r, w)

    def RMAX(out, in_, r, w):
        P.add("dve", lambda e: e.tensor_reduce(out=out, in_=in_, axis=AX.X, op=ALU.max), r, w)

    def RECIP(out, in_, r, w):
        P.add("dve", lambda e: e.reciprocal(out=out, in_=in_), r, w)

    def MEMSET(t, val, w, eng="pool"):
        P.add(eng, lambda e: e.memset(t, val), (), w)

    def ASEL(out, in_, pattern, cmp, fill, base, cm, r, w):
        P.add("pool", lambda e: e.affine_select(out=out, in_=in_, pattern=pattern, compare_op=cmp,
                                                fill=fill, base=base, channel_multiplier=cm), r, w)

    def DMA(eng, out, in_, r, w):
        P.add(eng, lambda e: e.dma_start(out=out, in_=in_), r, w, dma=True)

    ident = A.pers("ident", [128, 128], F32)
    onesD = A.pers("onesD", [128, 128], BF16)
    blk64 = A.pers("blk64", [128, 128], BF16)
    onesV = A.pers("onesV", [128, 128], BF16)
    ntri = A.pers("ntri", [128, 128], BF16)
    nones = A.pers("nones", [128, 128], BF16)
    gtri = A.pers("gtri", [128, 128], BF16)
    gstr = A.pers("gstr", [128, 128], BF16)
    minc = A.pers("minc", [128, 128], BF16)
    maskS = A.pers("maskS", [128, 4, 512], BF16)
    sel = A.pers("sel", [32, 32, 128], BF16)
    one11 = A.pers("one11", [1, 1], F32)
    c_col = A.pers("c_col", [128, KC], F32)
    c_act = A.pers("c_act", [128, KC], F32)
    modT = A.pers("modT", [128, DEPTH, 48], F32)
    gsm = A.pers("gsm", [128, DEPTH, KC], F32)
    gsf = A.pers("gsf", [128, DEPTH, KC], F32)
    nmc = A.pers("nmc", [128, DEPTH, KC], F32)
    nfc = A.pers("nfc", [128, DEPTH, KC], F32)
    badac = A.pers("badac", [128, DEPTH, 48], F32)
    qgc = A.pers("qgc", [128, DEPTH], F32)
    kgc = A.pers("kgc", [128, DEPTH], F32)
    ggc = A.pers("ggc", [128, DEPTH], F32)
    w2a = A.pers("w2a", [32, DEPTH, 256], BF16)
    rbT = A.pers("rbT", [36, DEPTH], F32)

    MEMSET(ident[:], 0.0, ["ident"])
    ASEL(ident[:], ident[:], [[-1, 128]], ALU.not_equal, 1.0, 0, 1, ["ident"], ["ident"])
    MEMSET(onesD[:], 1.0 / 1024.0, ["onesD"])
    MEMSET(onesV[:], 1.0 / 128.0, ["onesV"])
    MEMSET(nones[:], -1.0, ["nones"])
    MEMSET(one11[:], 1.0, ["one11"])
    MEMSET(blk64[:], 0.0, ["blk64"])
    MEMSET(blk64[0:64, 0:64], 1.0 / 64.0, ["blk64"])
    MEMSET(blk64[64:128, 64:128], 1.0 / 64.0, ["blk64"])
    MEMSET(ntri[:], -1.0, ["ntri"])
    ASEL(ntri[:], ntri[:], [[-1, 128]], ALU.is_ge, 0.0, 0, 1, ["ntri"], ["ntri"])
    MEMSET(gtri[:], -1.0 / 16.0, ["gtri"])
    ASEL(gtri[:], gtri[:], [[1, 128]], ALU.is_ge, 0.0, 0, -1, ["gtri"], ["gtri"])
    MEMSET(gstr[:], -1.0 / 16.0, ["gstr"])
    ASEL(gstr[:], gstr[:], [[-1, 128]], ALU.is_gt, 0.0, 0, 1, ["gstr"], ["gstr"])
    MEMSET(minc[:], 1.0, ["minc"])
    ASEL(minc[:], minc[:], [[1, 128]], ALU.is_ge, 0.0, 0, -1, ["minc"], ["minc"])
    MEMSET(maskS[:], 1.0, ["maskS"])
    ASEL(maskS[:], maskS[:], [[-128, 4], [1, 512]], ALU.is_gt, 0.0, 0, -1, ["maskS"], ["maskS"])
    MEMSET(sel[:], 0.0, ["sel"])
    ASEL(sel[:], sel[:], [[-1, 32], [0, 128]], ALU.not_equal, 1.0, 0, 1, ["sel"], ["sel"])

    DMA("sp", c_col[:], c_d, [], ["c_col"])
    DMA("sp", nmc[:], nm_d.rearrange("l p k -> p l k"), [], ["nmc"])
    DMA("sp", nfc[:], nf_d.rearrange("l p k -> p l k"), [], ["nfc"])
    DMA("sp", badac[:], bada_d.rearrange("l p k -> p l k"), [], ["badac"])
    DMA("sp", qgc[:], qg_d, [], ["qgc"])
    DMA("sp", kgc[:], kg_d, [], ["kgc"])
    DMA("sp", ggc[:], gg_d, [], ["ggc"])
    DMA("pool", w2a[:], w2_d.rearrange("l p n -> p l n"), [], ["w2a"])
    DMA("sp", rbT[:], br_d, [], ["rbT"])
    ACT(c_act[:], c_col[:], AF.Silu, ["c_col"], ["c_act"])
    TS(qgc[:], qgc[:], 0.125, None, ALU.mult, None, ["qgc"], ["qgc"])

    A.reset()
    xin = [A.ph(f"xin{i}", [128, 4, D], F32) for i in range(2)]
    xst = [A.ph(f"xst{i}", [128, KC, TG], F32) for i in range(2)]
    x_v = x_d.rearrange("(t p) d -> p t d", p=128)
    for g in range(NG):
        s = g % 2
        DMA("sp", xin[s][:], x_v[:, g * 4:(g + 1) * 4, :], [], [f"xin{s}"])
        for k in range(KC):
            b = k % 2
            for t in range(4):
                TR(banks[b][:, t * 128:(t + 1) * 128], xin[s][:, t, k * 128:(k + 1) * 128], ident[:],
                   [f"xin{s}", "ident"], [f"pb{b}"])
            if k % 2 == 0:
                CP(xst[s][:, k, :], banks[b][:, :], [f"pb{b}"], [f"xst{s}"])
            else:
                ACT(xst[s][:, k, :], banks[b][:, :], AF.Identity, [f"pb{b}"], [f"xst{s}"])
        DMA("sp", xT_v[:, :, g * TG:(g + 1) * TG], xst[s][:], [f"xst{s}"], ["xT"])
    P.barrier()

    A.reset()
    wa = [A.ph(f"wa{i}", [128, KC, 512], F32) for i in range(2)]
    mrow = [A.ph(f"mrow{i}", [1, 512], F32) for i in range(2)]
    cnt = 0
    for l in range(1):
        wa_v = wada_d[l].rearrange("(k p) n -> p k n", p=128)
        for cg in range(12):
            s = cnt % 2
            cnt += 1
            DMA("sp", wa[s][:], wa_v[:, :, cg * 512:(cg + 1) * 512], [], [f"wa{s}"])
            for k in range(KC):
                MM(banks[s][0:1, :], c_act[:, k:k + 1], wa[s][:, k, :], k == 0, k == KC - 1,
                   ["c_act", f"wa{s}"], [f"pb{s}"])
            CP(mrow[s][:], banks[s][0:1, :], [f"pb{s}"], [f"mrow{s}"])
            for i in range(4):
                j = cg * 4 + i
                MM(banks[2 + l % 2][:, j:j + 1], mrow[s][0:1, i * 128:(i + 1) * 128], one11[0:1, 0:1], True, True,
                   [f"mrow{s}", "one11"], [f"pmod{l % 2}"])
        TT(modT[:, l, :], banks[2 + l % 2][:, 0:48], badac[:, l, :], ALU.add, [f"pmod{l % 2}", "badac"], ["modT"])
        STT(gsm[:, l, :], modT[:, l, 8:16], 1.0, nmc[:, l, :], ALU.add, ALU.mult, ["modT", "nmc"], ["gsm"])
        STT(gsf[:, l, :], modT[:, l, 32:40], 1.0, nfc[:, l, :], ALU.add, ALU.mult, ["modT", "nfc"], ["gsf"])
    P.barrier()

    def norm_group(xs, xs_key, gs_ap, shift_ap, ms_bank, ms_key, sq, lnv, rstd, tmp32, emit_k):
        for k in range(KC):
            q = k % 2
            ACT(sq[q][:], xs(k), AF.Square, [xs_key], [f"sq{q}"])
            MM(ms_bank[:, :], onesD[:], sq[q][:], k == 0, k == KC - 1, ["onesD", f"sq{q}"], [ms_key])
        ACT(lnv[:], ms_bank[:, :], AF.Ln, [ms_key], ["lnv"], bias=EPS)
        ACT(rstd[:], lnv[:], AF.Exp, ["lnv"], ["rstd"], scale=-0.5)
        for k in range(KC):
            q = k % 2
            STT(tmp32[q][:], xs(k), gs_ap[:, k:k + 1], rstd[:], ALU.mult, ALU.mult,
                [xs_key, "rstd", "gsm", "gsf"], [f"tmp32{q}"])
            emit_k(k, tmp32[q], f"tmp32{q}", shift_ap[:, k:k + 1])

    for l in range(DEPTH):
        A.reset()
        win = A.ph("win", [128, KC, DIN], BF16)
        xg = [A.ph(f"xg{i}", [128, KC, TG], F32) for i in range(2)]
        hT = [A.ph(f"hT{i}", [128, KC, TG], BF16) for i in range(2)]
        sq = [A.ph(f"sq{i}", [128, TG], BF16) for i in range(4)]
        lnv = A.ph("lnv", [128, TG], F32)
        rstd = A.ph("rstd", [128, TG], F32)
        tmp32 = [A.ph(f"tmp32{i}", [128, TG], F32) for i in range(2)]
        sqh = [A.ph(f"sqh{i}", [128, TG], BF16) for i in range(2)]
        lnh = [A.ph(f"lnh{i}", [128, TG], F32) for i in range(2)]
        rsh = [A.ph(f"rsh{i}", [128, TG], F32) for i in range(2)]
        qkst = [A.ph(f"qkst{i}", [128, TG], BF16) for i in range(2)]
        vst = [A.ph("vst0", [128, 4, 512], BF16)] * 2
        gq = [A.ph(f"gq{i}", [128, 2, TG], BF16) for i in range(2)]
        gk = [A.ph(f"gk{i}", [128, 2, TG], BF16) for i in range(2)]
        grs = [A.ph(f"grs{i}", [128, 4, TG], BF16) for i in range(2)]
        glr = [A.ph(f"glr{i}", [32, TG], BF16) for i in range(2)]
        gkt = [A.ph(f"gkt{i}", [128, 4, 256], BF16) for i in range(2)]
        gv = [A.ph(f"gv{i}", [128, 4, 512], BF16) for i in range(2)]
        ge = A.ph("ge", [128, 256], F32)
        lt = [A.ph(f"lt{i}", [128, 256], BF16) for i in range(2)]
        eb = [A.ph(f"eb{i}", [128, 128], F32) for i in range(2)]
        enb = [A.ph(f"enb{i}", [128, 128], F32) for i in range(2)]
        erem = [A.ph(f"erem{i}", [128, 128], F32) for i in range(2)]
        qd = [A.ph(f"qd{i}", [128, 128], BF16) for i in range(2)]
        ki = [A.ph(f"ki{i}", [128, 128], BF16) for i in range(2)]
        kd = [A.ph(f"kd{i}", [128, 128], BF16) for i in range(2)]
        scm = [A.ph(f"scm{i}", [128, 128], BF16) for i in range(4)]
        state = [A.ph(f"state{i}", [128, 256], F32) for i in range(2)]
        stbf = [A.ph(f"stbf{i}", [128, 256], BF16) for i in range(2)]
        osq = A.ph("osq", [128, 512], BF16)
        oln = A.ph("oln", [128, 512], F32)
        ors = A.ph("ors", [128, 512], F32)
        on32 = A.ph("on32", [128, 512], F32)
        obg = [A.ph(f"obg{i}", [128, 4, TG], BF16) for i in range(2)]

        win_v = win_d[l].rearrange("(k p) n -> p k n", p=128)
        for c0_, c1_ in ((0, 512), (1024, 1536), (1536, 2048), (512, 1024), (2048, 2560), (2560, DIN)):
            DMA("pool", win[:, :, c0_:c1_], win_v[:, :, c0_:c1_], [], [f"win{c0_ // 512}"])
        for i in range(2):
            MEMSET(glr[i][:], 1.0, [f"glr{i}"])
        for hp in range(2):
            MEMSET(state[hp][:], 0.0, [f"state{hp}"])
            MEMSET(stbf[hp][:], 0.0, [f"stbf{hp}"])
        v_v4 = v_d.rearrange("(t p) c -> p t c", p=128)

        def pump(gen, n):
            if gen is None:
                return None
            for _ in range(n):
                try:
                    next(gen)
                except StopIteration:
                    return None
            return gen

        def gen_norm(g):
            s = g % 2
            gs_ = slice(g * TG, (g + 1) * TG)
            for k in range(4):
                ACT(sq[k][:], xg[s][:, k, :], AF.Square, [f"xg{s}"], [f"sq{k}"])
                if k % 2 == 1:
                    yield
            for k in range(4):
                MM(banks[3][:, :], onesD[:], sq[k][:], k == 0, False, ["onesD", f"sq{k}"], ["pb3"])
            for k in range(4, KC):
                ACT(sq[k - 4][:], xg[s][:, k, :], AF.Square, [f"xg{s}"], [f"sq{k - 4}"])
            for k in range(4, KC):
                MM(banks[3][:, :], onesD[:], sq[k - 4][:], False, k == KC - 1, ["onesD", f"sq{k - 4}"], ["pb3"])
            ACT(lnv[:], banks[3][:, :], AF.Ln, ["pb3"], ["lnv"], bias=EPS)
            ACT(rstd[:], lnv[:], AF.Exp, ["lnv"], ["rstd"], scale=-0.5)
            yield
            for k in range(KC):
                q = k % 2
                STT(tmp32[q][:], xg[s][:, k, :], gsm[:, l, k:k + 1], rstd[:], ALU.mult, ALU.mult,
                    [f"xg{s}", "rstd", "gsm"], [f"tmp32{q}"])
                ACT(hT[s][:, k, :], tmp32[q][:], AF.Identity, [f"tmp32{q}", "modT"], [f"hT{s}"],
                    bias=modT[:, l, k:k + 1])
                yield

        def gen_proj(g):
            s = g % 2
            gs_ = slice(g * TG, (g + 1) * TG)

            def fm_unit(kind, i, c0, b, q_):
                pb = banks[b]
                M = 16 if kind == "glr" else 128
                for k in range(KC):
                    MM(pb[0:M, :], win[:, k, c0:c0 + M], hT[s][:, k, :], k == 0, k == KC - 1,
                       [f"win{min(c0 // 512, 5)}", f"hT{s}"], [f"pb{b}"])
                if kind in ("q", "k"):
                    gain = qgc[:, l:l + 1] if kind == "q" else kgc[:, l:l + 1]
                    ACT(sqh[q_][:], pb[:, :], AF.Square, [f"pb{b}"], [f"sqh{q_}"])
                    MM(banks[3][:, :], blk64[:], sqh[q_][:], True, True, ["blk64", f"sqh{q_}"], ["pb3"])
                    ACT(lnh[q_][:], banks[3][:, :], AF.Ln, ["pb3"], [f"lnh{q_}"], bias=EPS)
                    ACT(rsh[q_][:], lnh[q_][:], AF.Exp, [f"lnh{q_}"], [f"rsh{q_}"], scale=-0.5)
                    STT(qkst[q_][:], pb[:, :], gain, rsh[q_][:], ALU.mult, ALU.mult,
                        [f"pb{b}", f"rsh{q_}", "qgc", "kgc"], [f"qkst{q_}"])
                    dst = qT_d if kind == "q" else kT_d
                    DMA("sp", dst[i * 128:(i + 1) * 128, gs_], qkst[q_][:], [f"qkst{q_}"], [f"{kind}T"])
                elif kind == "gq":
                    CP(gq[s][:, i, :], pb[:, :], [f"pb{b}"], [f"gq{s}"])
                elif kind == "gk":
                    CP(gk[s][:, i, :], pb[:, :], [f"pb{b}"], [f"gk{s}"])
                elif kind == "gr":
                    ACT(grs[s][:, i, :], pb[:, :], AF.Silu, [f"pb{b}"], [f"grs{s}"])
                else:
                    CP(glr[s][0:16, :], pb[0:16, :], [f"pb{b}"], [f"glr{s}"])

            def tm_unit(t, j, b):
                ts_ = slice(t * 128, (t + 1) * 128)
                c0, n, dstt, dkey = ((1024, 512, vst, "vst"), (1792, 256, gkt, "gkt"), (2048, 512, gv, "gv"))[j]
                for k in range(KC):
                    MM(banks[b][:, 0:n], hT[s][:, k, ts_], win[:, k, c0:c0 + n], k == 0, k == KC - 1,
                       [f"win{min(c0 // 512, 5)}", f"hT{s}"], [f"pb{b}"])
                okey = "vst0" if dkey == "vst" else f"{dkey}{s}"
                if j == 1:
                    CP(dstt[s][:, t, :], banks[b][:, 0:n], [f"pb{b}"], [okey])
                else:
                    ACT(dstt[s][:, t, :], banks[b][:, 0:n], AF.Identity, [f"pb{b}"], [okey])

            qk_units = [("q", i, i * 128) for i in range(4)] + [("k", i, 512 + i * 128) for i in range(4)]
            oth_units = ([("gq", i, 1536 + i * 128) for i in range(2)] + [("gk", i, 1792 + i * 128) for i in range(2)]
                         + [("gr", i, 2560 + i * 128) for i in range(4)] + [("glr", 0, 3072)])
            tm_units = [(t, j) for t in range(4) for j in range(3)]
            ti = 0
            for n_ in range(9):
                if n_ < 8:
                    kind, i, c0 = qk_units[n_]
                    fm_unit(kind, i, c0, 1, n_ % 2)
                    yield
                if ti < len(tm_units):
                    tm_unit(tm_units[ti][0], tm_units[ti][1], 4 if ti % 2 == 0 else 0)
                    ti += 1
                    yield
                kind, i, c0 = oth_units[n_]
                fm_unit(kind, i, c0, 2, 0)
                yield
            while ti < len(tm_units):
                tm_unit(tm_units[ti][0], tm_units[ti][1], 4 if ti % 2 == 0 else 0)
                ti += 1
                yield
            DMA("sp", v_v4[:, g * 4:(g + 1) * 4, :], vst[0][:], ["vst0"], ["v"])

        def gen_gla(g):
            s = g % 2
            gs_ = slice(g * TG, (g + 1) * TG)
            for t in range(4):
                ts_ = slice(t * 128, (t + 1) * 128)
                lq = t % 2
                MM(banks[3][:, 0:256], glr[s][0:32, ts_], w2a[0:32, l, :], True, True, [f"glr{s}", "w2a"], ["pb3"])
                ACT(ge[:], banks[3][:, 0:256], AF.Exp, ["pb3"], ["ge"], scale=-1.0)
                ACT(lt[lq][:], ge[:], AF.Ln, ["ge"], [f"lt{lq}"], bias=1.0)
                yield
                def chain(hp, bank, bkey):
                    hs_ = slice(hp * 128, (hp + 1) * 128)
                    MM(bank[:, 0:128], lt[lq][:, hs_], gtri[:], True, True, [f"lt{lq}", "gtri"], [bkey])
                    MM(bank[:, 128:256], gstr[:], lt[lq][:, hs_], True, True, [f"lt{lq}", "gstr"], [bkey])
                    ACT(eb[hp][:], bank[:, 0:128], AF.Exp, [bkey], [f"eb{hp}"])
                    ACT(enb[hp][:], bank[:, 0:128], AF.Exp, [bkey], [f"enb{hp}"], scale=-1.0)
                    ACT(erem[hp][:], bank[:, 128:256], AF.Exp, [bkey], [f"erem{hp}"])
                    yield
                    STT(qd[hp][:], eb[hp][:], 0.125, gq[s][:, hp, ts_], ALU.mult, ALU.mult,
                        [f"eb{hp}", f"gq{s}"], [f"qd{hp}"])
                    TT(ki[hp][:], enb[hp][:], gk[s][:, hp, ts_], ALU.mult, [f"enb{hp}", f"gk{s}"], [f"ki{hp}"])
                    TT(kd[hp][:], erem[hp][:], gkt[s][:, t, hs_], ALU.mult, [f"erem{hp}", f"gkt{s}"], [f"kd{hp}"])
                    yield
                    for a in range(2):
                        h = 2 * hp + a
                        pa = slice(a * 64, (a + 1) * 64)
                        scb = bank[:, 256 + a * 128:256 + (a + 1) * 128]
                        MM(scb, ki[hp][pa, :], qd[hp][pa, :], True, True, [f"ki{hp}", f"qd{hp}"], [bkey])
                        TT(scm[h][:], scb, minc[:], ALU.mult, [bkey, "minc"], [f"scm{h}"])
                        yield
                        ob = banks[7][:, h * 128:(h + 1) * 128]
                        MM(ob, gv[s][:, t, h * 128:(h + 1) * 128], scm[h][:], True, False,
                           [f"gv{s}", f"scm{h}"], ["pb7"])
                        MM(ob, stbf[hp][pa, a * 128:(a + 1) * 128], qd[hp][pa, :], False, True,
                           [f"stbf{hp}", f"qd{hp}"], ["pb7"])
                    MM(bank[:, 0:256], kd[hp][:], gv[s][:, t, hp * 256:(hp + 1) * 256], True, True,
                       [f"kd{hp}", f"gv{s}"], [bkey])
                    STT(state[hp][:], state[hp][:], eb[hp][:, 127:128], bank[:, 0:256], ALU.mult, ALU.add,
                        [f"state{hp}", f"eb{hp}", bkey], [f"state{hp}"])
                    ACT(stbf[hp][:], state[hp][:], AF.Identity, [f"state{hp}"], [f"stbf{hp}"])
                    yield

                c0_, c1_ = chain(0, banks[6], "pb6"), chain(1, banks[5], "pb5")
                while c0_ is not None or c1_ is not None:
                    c0_ = pump(c0_, 1)
                    c1_ = pump(c1_, 1)
                    yield
                ACT(osq[:], banks[7][:, :], AF.Square, ["pb7"], ["osq"])
                MM(banks[3][:, :], onesV[:], osq[:], True, True, ["onesV", "osq"], ["pb3"])
                ACT(oln[:], banks[3][:, :], AF.Ln, ["pb3"], ["oln"], bias=EPS)
                ACT(ors[:], oln[:], AF.Exp, ["oln"], ["ors"], scale=-0.5)
                yield
                STT(on32[:], banks[7][:, :], ggc[:, l:l + 1], ors[:], ALU.mult, ALU.mult,
                    ["pb7", "ors", "ggc"], ["on32"])
                for h in range(4):
                    TT(obg[s][:, h, ts_], on32[:, h * 128:(h + 1) * 128], grs[s][:, h, ts_], ALU.mult,
                       ["on32", f"grs{s}"], [f"obg{s}"], eng="pool")
                yield
            DMA("sp", mixT_v[:, 4:8, gs_], obg[s][:], [f"obg{s}"], ["mixT"])

        def load_xg(g):
            DMA("sp", xg[g % 2][:], xT_v[:, :, g * TG:(g + 1) * TG], ["xT"], [f"xg{g % 2}"])

        load_xg(0)
        if NG > 1:
            load_xg(1)
        for step in range(-1, NG + 1):
            if step >= 0 and step + 2 < NG:
                load_xg(step + 2)
            gn = gen_norm(step + 1) if 0 <= step + 1 < NG else None
            gp = gen_proj(step) if 0 <= step < NG else None
            gl_ = gen_gla(step - 1) if 0 <= step - 1 < NG else None
            cyc = 0
            while gn is not None or gp is not None or gl_ is not None:
                gp = pump(gp, 1)
                gl_ = pump(gl_, 1)
                cyc += 1
                if cyc % 4 == 0 or gp is None:
                    gl_ = pump(gl_, 1)
                if cyc % 2 == 0 or gp is None:
                    gn = pump(gn, 1)
        P.barrier()

        A.reset()
        WOUT_OFF = 204480
        wout = nc.alloc_sbuf_tensor_at(f"wout_l{l}", [128, KC, D], BF16, offset=WOUT_OFF)
        wr = nc.alloc_sbuf_tensor_at(f"wr_l{l}", [128, KC, 36], F32, offset=WOUT_OFF + 16384)
        DMA("pool", wout[:], wout_d[l].rearrange("(k p) n -> p k n", p=128), [], ["wout"])
        DMA("sp", wr[:], wr_d[l].rearrange("(k p) n -> p k n", p=128), [], ["wr"])
        QT = [A.ph(f"QT{i}", [128, S], BF16) for i in range(2)]
        KT = [A.ph(f"KT{i}", [128, S], BF16) for i in range(2)]
        Vp = [A.ph(f"Vp{i}", [128, NT, 2, 128], BF16) for i in range(2)]
        ez = [A.ph(f"ez{i}", [128, TG], F32) for i in range(2)]
        spb = [A.ph(f"spb{i}", [128, TG], BF16) for i in range(4)]
        wb = [A.ph(f"wb{i}", [128, TG], BF16) for i in range(4)]
        lacc = [A.ph(f"lacc{i}", [128, TG], BF16) for i in range(2)]
        oast = [A.ph(f"oast{i}", [128, TG], BF16) for i in range(2)]
        for i in range(2):
            MEMSET(Vp[i][:], 0.0, [f"Vp{i}"])
        v_v = v_d.rearrange("(t p) c -> p t c", p=128)
        L2 = l + 1
        do_mod = L2 < DEPTH
        modst = {"next": 0, "steps": 0}
        if do_mod:
            wa2 = [A.ph(f"wa2_{i}", [128, KC, 256], F32) for i in range(2)]
            mrow2 = [A.ph(f"mrow2_{i}", [1, 256], F32) for i in range(2)]
            wa_v2 = wada_d[L2].rearrange("(k p) n -> p k n", p=128)
            MOD_EVERY = max(1, (4 * 2 * sum(4 * g_ + 4 for g_ in range(NG))) // 26)

            def mod_load(cg):
                DMA("sp", wa2[cg % 2][:], wa_v2[:, :, cg * 256:(cg + 1) * 256], [], [f"wa2_{cg % 2}"])

            def mod_chunk(cg):
                sl = cg % 2
                for k in range(KC):
                    MM(banks[7][0:1, 256:512], c_act[:, k:k + 1], wa2[sl][:, k, :], k == 0, k == KC - 1,
                       ["c_act", f"wa2_{sl}"], ["pb7"])
                CP(mrow2[sl][:], banks[7][0:1, 256:512], ["pb7"], [f"mrow2_{sl}"])
                for i in range(2):
                    j = cg * 2 + i
                    MM(banks[7][:, j:j + 1], mrow2[sl][0:1, i * 128:(i + 1) * 128], one11[0:1, 0:1], True, True,
                       [f"mrow2_{sl}", "one11"], ["pb7"])
                if cg + 2 < 24:
                    mod_load(cg + 2)

            def mod_finish():
                while modst["next"] < 24:
                    mod_chunk(modst["next"])
                    modst["next"] += 1
                TT(modT[:, L2, :], banks[7][:, 0:48], badac[:, L2, :], ALU.add, ["pb7", "badac"], ["modT"])
                STT(gsm[:, L2, :], modT[:, L2, 8:16], 1.0, nmc[:, L2, :], ALU.add, ALU.mult, ["modT", "nmc"], ["gsm"])
                STT(gsf[:, L2, :], modT[:, L2, 32:40], 1.0, nfc[:, L2, :], ALU.add, ALU.mult, ["modT", "nfc"], ["gsf"])

            mod_load(0)
            mod_load(1)
        for hp in range(4):
            s = hp % 2
            DMA("sp", QT[s][:], qT_d[hp * 128:(hp + 1) * 128, :], ["qT"], [f"QT{s}"])
            DMA("sp", KT[s][:], kT_d[hp * 128:(hp + 1) * 128, :], ["kT"], [f"KT{s}"])
            for a in range(2):
                h = 2 * hp + a
                for t0 in range(0, NT, 8):
                    t1 = min(NT, t0 + 8)
                    DMA("sp", Vp[s][:, t0:t1, a, a * 64:(a + 1) * 64], v_v[:, t0:t1, h * 64:(h + 1) * 64],
                        ["v"], [f"Vp{s}"])
            steps = []
            for g in range(NG):
                for kt in range(4 * g + 3, -1, -1):
                    for a in range(2):
                        steps.append((g, kt, a))
            NS = len(steps)

            def info(i):
                g, kt, a = steps[i]
                r = kt - 4 * g
                c0 = max(r, 0) * 128
                return g, kt, a, r, c0

            def st_z(i):
                g, kt, a, r, c0 = info(i)
                zq = i % 2
                pa = slice(a * 64, (a + 1) * 64)
                MM(banks[zq][:, c0:TG], KT[s][pa, kt * 128:(kt + 1) * 128], QT[s][pa, g * TG + c0:(g + 1) * TG],
                   True, True, [f"KT{s}", f"QT{s}"], [f"pb{zq}"])

            def st_ez(i):
                g, kt, a, r, c0 = info(i)
                zq = i % 2
                ACT(ez[zq][:, c0:TG], banks[zq][:, c0:TG], AF.Exp, [f"pb{zq}"], [f"ez{zq}"])

            def st_ln(i):
                g, kt, a, r, c0 = info(i)
                zq = i % 2
                s4 = i % 4
                ACT(spb[s4][:, c0:TG], ez[zq][:, c0:TG], AF.Ln, [f"ez{zq}"], [f"spb{s4}"], bias=1.0)
                if r >= 0:
                    TT(spb[s4][:, c0:TG], spb[s4][:, c0:TG], maskS[:, r, c0:TG], ALU.mult,
                       [f"spb{s4}", "maskS"], [f"spb{s4}"])

            def st_lw(i):
                g, kt, a, r, c0 = info(i)
                s4 = i % 4
                lq = 2 + i % 3
                pa = slice(a * 64, (a + 1) * 64)
                fog = (kt == 4 * g + 3)
                lb = banks[lq]
                MM(lb[:, c0:TG], KT[s][pa, kt * 128:(kt + 1) * 128], QT[s][pa, g * TG + c0:(g + 1) * TG],
                   True, False, [f"KT{s}", f"QT{s}"], [f"pb{lq}"])
                MM(lb[:, c0:TG], ntri[:], spb[s4][:, c0:TG], False, fog, ["ntri", f"spb{s4}"], [f"pb{lq}"])
                if not fog:
                    MM(lb[:, c0:TG], nones[:], lacc[a][:, c0:TG], False, True, ["nones", f"lacc{a}"], [f"pb{lq}"])
                if kt > 0:
                    if fog:
                        if c0 > 0:
                            MEMSET(lacc[a][:, 0:c0], 0.0, [f"lacc{a}"])
                        CP(lacc[a][:, c0:TG], spb[s4][:, c0:TG], [f"spb{s4}"], [f"lacc{a}"])
                    else:
                        TT(lacc[a][:, c0:TG], lacc[a][:, c0:TG], spb[s4][:, c0:TG], ALU.add,
                           [f"lacc{a}", f"spb{s4}"], [f"lacc{a}"])

            def st_ew(i):
                g, kt, a, r, c0 = info(i)
                lq = 2 + i % 3
                w4 = i % 4
                ACT(wb[w4][:, c0:TG], banks[lq][:, c0:TG], AF.Exp, [f"pb{lq}"], [f"wb{w4}"])
                if r >= 0:
                    TT(wb[w4][:, c0:TG], wb[w4][:, c0:TG], maskS[:, r, c0:TG], ALU.mult,
                       [f"wb{w4}", "maskS"], [f"wb{w4}"])

            def st_acc(i):
                g, kt, a, r, c0 = info(i)
                w4 = i % 4
                ab = 5 + g % 2
                first = (kt == 4 * g + 3 and a == 0)
                last = (kt == 0 and a == 1)
                out_ap, lhsT_ap, rhs_ap = banks[ab][:, c0:TG], Vp[s][:, kt, a, :], wb[w4][:, c0:TG]
                P.add("pe", lambda e: e.matmul(out_ap, lhsT=lhsT_ap, rhs=rhs_ap, start=first, stop=last,
                                               skip_group_check=True),
                      [f"Vp{s}", f"wb{w4}"], [f"pb{ab}"])
                if last:
                    o = g % 2
                    CP(oast[o][:], banks[ab][:, :], [f"pb{ab}"], [f"oast{o}"])
                    DMA("sp", mixT_v[:, hp, g * TG:(g + 1) * TG], oast[o][:], [f"oast{o}"], ["mixT"])

            for i in range(-2, NS + 2):
                if do_mod:
                    modst["steps"] += 1
                    if modst["steps"] % MOD_EVERY == 0 and modst["next"] < 24:
                        mod_chunk(modst["next"])
                        modst["next"] += 1
                if 0 <= i + 2 < NS:
                    st_z(i + 2)
                if 0 <= i + 1 < NS:
                    st_ez(i + 1)
                if 0 <= i < NS:
                    st_lw(i)
                if 0 <= i - 1 < NS:
                    st_ew(i - 1)
                if 0 <= i + 1 < NS:
                    st_ln(i + 1)
                if 0 <= i - 2 < NS:
                    st_acc(i - 2)
        if do_mod:
            mod_finish()
        P.barrier()

        A.reset()
        x1 = A.ph("x1", [128, KC, HS], F32)
        h2T = A.ph("h2T", [128, KC, HS], BF16)
        combT = A.ph("combT", [32, HS], BF16)
        NW = 3
        wgb = [A.ph(f"wgb{i}", [128, KC, 256], BF16) for i in range(NW)]
        wub = [A.ph(f"wub{i}", [128, KC, 256], BF16) for i in range(NW)]
        wdb = [A.ph(f"wdb{i}", [128, 2, D], BF16) for i in range(NW)]
        mark = A.cur
        mix = [A.ph(f"mix{i}", [128, KC, TG], BF16) for i in range(1)]
        h32 = [A.ph(f"h32{i}", [128, TG], F32) for i in range(2)]
        lgT = A.ph("lgT", [36, TG], F32)
        sq = [A.ph(f"sq{i}", [128, TG], BF16) for i in range(2)]
        lnv = A.ph("lnv", [128, TG], F32)
        rstd = A.ph("rstd", [128, TG], F32)
        tmp32 = [A.ph(f"tmp32{i}", [128, TG], F32) for i in range(2)]
        top_d = A.cur
        A.cur = mark
        bcs = [A.ph(f"bcs{i}", [128, TG], F32) for i in range(2)]
        sil = [A.ph(f"sil{i}", [128, TG], F32) for i in range(2)]
        su = [A.ph(f"su{i}", [128, TG], F32) for i in range(2)]
        actb = [A.ph(f"actb{i}", [128, TG], BF16) for i in range(8)]
        A.cur = max(A.cur, top_d)
        rt4 = {n: A.ph(f"rt_{n}", [128, 4, w_], F32) for n, w_ in
              (("lg", 36), ("gmax", 1), ("ngmax", 1), ("ohg", 4), ("gex", 4), ("gsum", 1), ("grw", 1), ("pen", 4),
               ("elm", 32), ("m1", 1), ("oh1", 32), ("elm2", 32), ("m2", 1), ("oh2", 32), ("dd", 1), ("e2", 1),
               ("w1", 1), ("w1g", 1), ("w2g", 1), ("c1", 32), ("comb", 32))}
        rts = [{n: v[:, t, :] for n, v in rt4.items()} for t in range(4)]

        assert A.cur <= WOUT_OFF, A.cur

        def gen_op(half, gg):
            g = half * GH + gg
            gs_ = slice(g * TG, (g + 1) * TG)
            ls_ = slice(gg * TG, (gg + 1) * TG)
            xk = f"x1_{gg}"
            DMA("sp", mix[0][:], mixT_v[:, :, gs_], ["mixT"], ["mix0"])
            for oc in range(KC):
                b = oc % 2
                for k in range(KC):
                    MM(banks[b][:, :], wout[:, k, oc * 128:(oc + 1) * 128], mix[0][:, k, :], k == 0, k == KC - 1,
                       ["wout", "mix0"], [f"pb{b}"])
                STT(x1[:, oc, ls_], banks[b][:, :], modT[:, l, 16 + oc:17 + oc], x1[:, oc, ls_], ALU.mult, ALU.add,
                    [f"pb{b}", xk, "modT"], [xk])
                yield

        def gen_n2(half, gg):
            ls_ = slice(gg * TG, (gg + 1) * TG)
            xk = f"x1_{gg}"
            for k in range(KC):
                q = k % 2
                ACT(sq[q][:], x1[:, k, ls_], AF.Square, [xk], [f"sq{q}"])
                MM(banks[2][:, :], onesD[:], sq[q][:], k == 0, k == KC - 1, ["onesD", f"sq{q}"], ["pb2"])
                yield
            ACT(lnv[:], banks[2][:, :], AF.Ln, ["pb2"], ["lnv"], bias=EPS)
            ACT(rstd[:], lnv[:], AF.Exp, ["lnv"], ["rstd"], scale=-0.5)
            yield
            for k in range(KC):
                q = k % 2
                STT(tmp32[q][:], x1[:, k, ls_], gsf[:, l, k:k + 1], rstd[:], ALU.mult, ALU.mult,
                    [xk, "rstd", "gsf"], [f"tmp32{q}"])
                ACT(h32[q][:], tmp32[q][:], AF.Identity, [f"tmp32{q}", "modT"], [f"h32{q}"], bias=modT[:, l, 24 + k:25 + k])
                MM(banks[3][0:36, :], wr[:, k, :], h32[q][:], k == 0, k == KC - 1, ["wr", f"h32{q}"], ["pb3"])
                CP(h2T[:, k, ls_], h32[q][:], [f"h32{q}"], [f"h2T_{gg}"], eng="pool")
                yield
            ACT(lgT[:], banks[3][0:36, :], AF.Identity, ["pb3", "rbT"], ["lgT"], bias=rbT[0:36, l:l + 1])

        def rt_chain(t, ts_):
            R = rts[t]
            K_ = lambda n: f"{n}{t}"
            CP(R["lg"], banks[4][:, t * 36:(t + 1) * 36], ["pb4"], [K_("lg")])
            yield
            RMAX(R["gmax"], R["lg"][:, 0:4], [K_("lg")], [K_("gmax")])
            yield
            TS(R["ohg"], R["lg"][:, 0:4], R["gmax"][:, 0:1], None, ALU.is_equal, None, [K_("lg"), K_("gmax")], [K_("ohg")])
            TS(R["ngmax"], R["gmax"], -1.0, None, ALU.mult, None, [K_("gmax")], [K_("ngmax")])
            yield
            ACT(R["gex"], R["lg"][:, 0:4], AF.Exp, [K_("lg"), K_("ngmax")], [K_("gex"), K_("gsum")],
                bias=R["ngmax"][:, 0:1], accum_out=R["gsum"][:, 0:1])
            TS(R["pen"], R["ohg"], BIG, -BIG, ALU.mult, ALU.add, [K_("ohg")], [K_("pen")])
            yield
            RECIP(R["grw"], R["gsum"], [K_("gsum")], [K_("grw")])
            for gi in range(4):
                TS(R["elm"][:, gi * 8:(gi + 1) * 8], R["lg"][:, 4 + gi * 8:12 + gi * 8], R["pen"][:, gi:gi + 1],
                   None, ALU.add, None, [K_("lg"), K_("pen")], [K_("elm")])
            yield
            RMAX(R["m1"], R["elm"], [K_("elm")], [K_("m1")])
            yield
            TS(R["oh1"], R["elm"], R["m1"][:, 0:1], None, ALU.is_equal, None, [K_("elm"), K_("m1")], [K_("oh1")])
            yield
            STT(R["elm2"], R["oh1"], -BIG, R["elm"], ALU.mult, ALU.add, [K_("oh1"), K_("elm")], [K_("elm2")])
            yield
            RMAX(R["m2"], R["elm2"], [K_("elm2")], [K_("m2")])
            yield
            TS(R["oh2"], R["elm2"], R["m2"][:, 0:1], None, ALU.is_equal, None, [K_("elm2"), K_("m2")], [K_("oh2")])
            TT(R["dd"], R["m2"], R["m1"], ALU.subtract, [K_("m1"), K_("m2")], [K_("dd")])
            yield
            ACT(R["e2"], R["dd"], AF.Exp, [K_("dd")], [K_("e2")])
            yield
            TS(R["w1"], R["e2"], 1.0, None, ALU.add, None, [K_("e2")], [K_("w1")])
            yield
            RECIP(R["w1"], R["w1"], [K_("w1")], [K_("w1")])
            yield
            TT(R["w1g"], R["w1"], R["grw"], ALU.mult, [K_("w1"), K_("grw")], [K_("w1g")])
            yield
            TT(R["w2g"], R["e2"], R["w1g"], ALU.mult, [K_("e2"), K_("w1g")], [K_("w2g")])
            TS(R["c1"], R["oh1"], R["w1g"][:, 0:1], None, ALU.mult, None, [K_("oh1"), K_("w1g")], [K_("c1")])
            yield
            STT(R["comb"], R["oh2"], R["w2g"][:, 0:1], R["c1"], ALU.mult, ALU.add,
                [K_("oh2"), K_("w2g"), K_("c1")], [K_("comb")])
            yield
            TR(banks[5][0:32, ts_], R["comb"], ident[:], [K_("comb"), "ident"], ["pb5"])

        def gen_rt(half, gg):
            ls_ = slice(gg * TG, (gg + 1) * TG)
            for t in range(4):
                TR(banks[4][:, t * 36:(t + 1) * 36], lgT[0:36, t * 128:(t + 1) * 128], ident[0:36, 0:36],
                   ["lgT", "ident"], ["pb4"])
            yield
            chains = [rt_chain(t, slice(t * 128, (t + 1) * 128)) for t in range(4)]
            while any(c is not None for c in chains):
                chains = [pump2(c) for c in chains]
                yield
            CP(combT[0:32, ls_], banks[5][0:32, :], ["pb5"], ["combT"])

        def pump2(gen):
            if gen is None:
                return None
            try:
                next(gen)
            except StopIteration:
                return None
            return gen

        wstate = {"cnt": 0}

        def issue_weights(ep):
            slots = []
            for e_ in (2 * ep, 2 * ep + 1):
                ws = wstate["cnt"] % NW
                wstate["cnt"] += 1
                slots.append(ws)
                DMA("pool", wgb[ws][:], wg_d[l, e_].rearrange("(k p) f -> p k f", p=128), [], [f"wgb{ws}"])
                DMA("pool", wub[ws][:], wu_d[l, e_].rearrange("(k p) f -> p k f", p=128), [], [f"wub{ws}"])
                DMA("pool", wdb[ws][:], wd_d[l, e_].rearrange("(c p) n -> p c n", p=128), [], [f"wdb{ws}"])
            return slots

        for half in range(NHALF):
            slots0 = issue_weights(0)
            for gg_ in range(GH):
                g_ = half * GH + gg_
                DMA("sp", x1[:, :, gg_ * TG:(gg_ + 1) * TG], xT_v[:, :, g_ * TG:(g_ + 1) * TG], ["xT"], [f"x1_{gg_}"])
            for step in range(-1, GH + 1):
                go = gen_op(half, step + 1) if 0 <= step + 1 < GH else None
                gn2 = gen_n2(half, step) if 0 <= step < GH else None
                gr = gen_rt(half, step - 1) if 0 <= step - 1 < GH else None
                while go is not None or gn2 is not None or gr is not None:
                    go = pump2(go)
                    gn2 = pump2(gn2)
                    gn2 = pump2(gn2)
                    gr = pump2(gr)
                    gr = pump2(gr)
            P.barrier()
            def gate_up(ep, slots, gg, ab0):
                ls_ = slice(gg * TG, (gg + 1) * TG)
                for ei, e_ in enumerate((2 * ep, 2 * ep + 1)):
                    ws = slots[ei]
                    MM(banks[0][:, :], sel[0:32, e_, :], combT[0:32, ls_], True, True, ["sel", "combT"], ["pb0"])
                    ACT(bcs[ei][:], banks[0][:, :], AF.Identity, ["pb0"], [f"bcs{ei}"])
                    for fc in range(2):
                        fs_ = slice(fc * 128, (fc + 1) * 128)
                        q = fc
                        for k in range(KC):
                            MM(banks[1 + q][:, :], wgb[ws][:, k, fs_], h2T[:, k, ls_], k == 0, k == KC - 1,
                               [f"wgb{ws}", "h2T"], [f"pb{1 + q}"])
                        for k in range(KC):
                            MM(banks[3 + q][:, :], wub[ws][:, k, fs_], h2T[:, k, ls_], k == 0, k == KC - 1,
                               [f"wub{ws}", "h2T"], [f"pb{3 + q}"])
                        ACT(sil[q][:], banks[1 + q][:, :], AF.Silu, [f"pb{1 + q}"], [f"sil{q}"])
                        TT(su[q][:], sil[q][:], banks[3 + q][:, :], ALU.mult, [f"sil{q}", f"pb{3 + q}"], [f"su{q}"])
                        ai = ab0 + ei * 2 + fc
                        TT(actb[ai][:], su[q][:], bcs[ei][:], ALU.mult, [f"su{q}", f"bcs{ei}"], [f"actb{ai}"])

            def down(ep, slots, gg, ab0):
                ls_ = slice(gg * TG, (gg + 1) * TG)
                for oc in range(KC):
                    b = 5 + oc % 2
                    n = 0
                    for ei in range(2):
                        ws = slots[ei]
                        for fc in range(2):
                            ai = ab0 + ei * 2 + fc
                            MM(banks[b][:, :], wdb[ws][:, fc, oc * 128:(oc + 1) * 128], actb[ai][:], n == 0, n == 3,
                               [f"wdb{ws}", f"actb{ai}"], [f"pb{b}"])
                            n += 1
                    STT(x1[:, oc, ls_], banks[b][:, :], modT[:, l, 40 + oc:41 + oc], x1[:, oc, ls_],
                        ALU.mult, ALU.add, [f"pb{b}", f"x1_{gg}", "modT"], [f"x1_{gg}"])
                if ep == 15:
                    g_ = half * GH + gg
                    DMA("sp", xT_v[:, :, g_ * TG:(g_ + 1) * TG], x1[:, :, ls_], [f"x1_{gg}"], ["xT"])

            pending = None
            nset = 0
            for ep in range(16):
                slots = slots0 if ep == 0 else issue_weights(ep)
                for gg in range(GH):
                    ab0 = (nset % 2) * 4
                    nset += 1
                    gate_up(ep, slots, gg, ab0)
                    if pending is not None:
                        down(*pending)
                    pending = (ep, slots, gg, ab0)
                down(*pending)
                pending = None
            P.barrier()

    A.reset()
    xe = [A.ph(f"xe{i}", [128, KC, TG], F32) for i in range(2)]
    ost = [A.ph(f"ost{i}", [128, 4, D], F32) for i in range(2)]
    out_v = out_d.rearrange("(t p) d -> p t d", p=128)
    for g in range(NG):
        s = g % 2
        DMA("sp", xe[s][:], xT_v[:, :, g * TG:(g + 1) * TG], ["xT"], [f"xe{s}"])
        for t in range(4):
            for hk in range(2):
                b = (t * 2 + hk) % 2
                for kk in range(4):
                    k = hk * 4 + kk
                    TR(banks[b][:, kk * 128:(kk + 1) * 128], xe[s][:, k, t * 128:(t + 1) * 128], ident[:],
                       [f"xe{s}", "ident"], [f"pb{b}"])
                if hk == 0:
                    CP(ost[s][:, t, 0:512], banks[b][:, :], [f"pb{b}"], [f"ost{s}"])
                else:
                    ACT(ost[s][:, t, 512:1024], banks[b][:, :], AF.Identity, [f"pb{b}"], [f"ost{s}"])
        DMA("sp", out_v[:, g * 4:(g + 1) * 4, :], ost[s][:], [f"ost{s}"], ["out"])
    P.emit()
    return nc


def _col(v, n):
    return np.ascontiguousarray(np.asarray(v, dtype=np.float32).reshape(n, 128).T)


def make_in_maps(inputs, S, DEPTH, n_cores):
    f = lambda a: np.ascontiguousarray(np.asarray(a, dtype=np.float32))
    L = DEPTH
    shared = {
        "w_ada": f(inputs["w_ada"][:L]),
        "b_ada_col": np.stack([_col(inputs["b_ada"][l], 48) for l in range(L)]),
        "nm_col": np.stack([_col(inputs["norm_mix"][l], KC) for l in range(L)]),
        "nf_col": np.stack([_col(inputs["norm_ffn"][l], KC) for l in range(L)]),
        "w_in": f(inputs["w_in"][:L]),
        "w_out": f(inputs["w_out"][:L]),
        "w_gate": f(np.asarray(inputs["w_gate"][:L]).reshape(L, 32, D, 256)),
        "w_up": f(np.asarray(inputs["w_up"][:L]).reshape(L, 32, D, 256)),
        "w_down": f(np.asarray(inputs["w_down"][:L]).reshape(L, 32, 256, D)),
    }
    w2aug = np.zeros((L, 32, 256), np.float32)
    w2aug[:, 0:16, :] = np.asarray(inputs["w_gk2"][:L])
    w2aug[:, 16, :] = np.asarray(inputs["b_gk"][:L])
    shared["w2aug"] = w2aug
    rep2 = lambda v: np.concatenate([v, v]).astype(np.float32)
    shared["qg_col"] = f(np.stack([rep2(np.asarray(inputs["q_gain"][l])) for l in range(L)], axis=1))
    shared["kg_col"] = f(np.stack([rep2(np.asarray(inputs["k_gain"][l])) for l in range(L)], axis=1))
    shared["gg_col"] = f(np.stack([np.asarray(inputs["gla_gain"][l], np.float32) for l in range(L)], axis=1))
    shared["w_r"] = f(np.concatenate([np.asarray(inputs["w_router_grp"][:L]), np.asarray(inputs["w_router_exp"][:L])], axis=2))
    b_r = np.concatenate([np.asarray(inputs["b_router_grp"][:L]), np.asarray(inputs["b_router_exp"][:L])], axis=1)
    shared["b_rT"] = f(b_r.T)
    maps = []
    x = np.asarray(inputs["x"], dtype=np.float32)
    c = np.asarray(inputs["c"], dtype=np.float32)
    for b in range(n_cores):
        m = dict(shared)
        m["x"] = np.ascontiguousarray(x[b, :S])
        m["c_col"] = _col(c[b], KC)
        maps.append(m)
    return maps


_NC_CACHE = {}


def kernel(**inputs):
    x = np.asarray(inputs["x"])
    B, S, _ = x.shape
    DEPTH = np.asarray(inputs["w_ada"]).shape[0]
    key = (S, DEPTH)
    if key not in _NC_CACHE:
        _NC_CACHE[key] = build(S, DEPTH)
    nc = _NC_CACHE[key]
    maps = make_in_maps(inputs, S, DEPTH, B)
    res = run_bass_kernel_spmd(nc, maps, core_ids=list(range(B)))
    return np.stack([np.asarray(r["out"], dtype=np.float32) for r in res.results], axis=0)
```

```python
import numpy as np
from contextlib import ExitStack
import concourse.bass as bass
import concourse.mybir as mybir
from concourse.bass_utils import run_bass_kernel_spmd

F32 = mybir.dt.float32
BF16 = mybir.dt.bfloat16
AF = mybir.ActivationFunctionType
ALU = mybir.AluOpType
AX = mybir.AxisListType

D = 1024
KC = 8
DIN = 3088
NMOD = 6 * D
EPS = 1e-6
BIG = 1.0e30
TG = 512

ENGS = ("pe", "act", "dve", "pool", "sp")
TICK_LIMIT = 30000


class Op:
    __slots__ = ("eng", "fn", "deps", "dma", "needed", "tick", "dsem", "dval")

    def __init__(self, eng, fn, dma):
        self.eng = eng
        self.fn = fn
        self.deps = []
        self.dma = dma
        self.needed = False
        self.tick = None
        self.dsem = None
        self.dval = None


class Prog:
    def __init__(self, nc, n_dma_sems=40, sems_per_eng=12):
        self.nc = nc
        self.ops = {e: [] for e in ENGS}
        self.last_w = {}
        self.readers = {}
        self.n_dma_sems = n_dma_sems
        self.sems_per_eng = sems_per_eng
        self.n_sw = 10
        self.n_hw = n_dma_sems - self.n_sw
        self.dma_count = 0
        self.sw_count = 0
        self.recent_dmas = []
        self.recent_sw = []

    def add(self, eng, fn, reads=(), writes=(), dma=False):
        op = Op(eng, fn, dma)
        deps = set()
        for r in reads:
            w = self.last_w.get(r)
            if w is not None:
                deps.add(w)
        for w_ in writes:
            w = self.last_w.get(w_)
            if w is not None:
                deps.add(w)
            for rd in self.readers.get(w_, ()):
                deps.add(rd)
        for r in reads:
            self.readers.setdefault(r, []).append(op)
        for w_ in writes:
            self.last_w[w_] = op
            self.readers[w_] = []
        deps.discard(op)
        for d in deps:
            if (not d.dma) and d.eng == "pe" and eng == "pe" and not dma:
                continue
            op.deps.append(d)
            d.needed = True
        if dma:
            if eng == "pool":
                op.dsem = self.n_hw + self.sw_count % self.n_sw
                op.dval = 16 * (self.sw_count // self.n_sw + 1)
                self.sw_count += 1
                self.recent_sw.append(op)
                if len(self.recent_sw) > self.n_sw:
                    self.recent_sw.pop(0)
            else:
                op.dsem = self.dma_count % self.n_hw
                op.dval = 16 * (self.dma_count // self.n_hw + 1)
                self.dma_count += 1
                self.recent_dmas.append(op)
                if len(self.recent_dmas) > self.n_hw:
                    self.recent_dmas.pop(0)
        self.ops[eng].append(op)
        return op

    def barrier(self):
        marks = []
        for e in ENGS:
            for o in reversed(self.ops[e]):
                if not o.dma and o.fn is not None:
                    marks.append(o)
                    break
        marks = [m for m in marks if m.eng != "sp"]
        dmas = list(self.recent_dmas) + list(self.recent_sw)
        for e in ENGS:
            nop = Op(e, None, False)
            for d in marks + dmas:
                nop.deps.append(d)
                d.needed = True
            self.ops[e].append(nop)
        self.last_w = {}
        self.readers = {}

    def emit(self):
        nc = self.nc
        with ExitStack() as es:
            esems = {e: [es.enter_context(nc.semaphore(f"t_{e}_{i}")) for i in range(self.sems_per_eng)]
                     for e in ENGS if e != "sp"}
            dsems = [es.enter_context(nc.semaphore(f"d_{i}")) for i in range(self.n_dma_sems)]
            for e in ENGS:
                cnt = 0
                epoch = 0
                for op in self.ops[e]:
                    if op.dma or not op.needed:
                        continue
                    if e == "sp":
                        raise RuntimeError("sp compute op cannot be depended on")
                    cnt += 1
                    if cnt > TICK_LIMIT:
                        epoch += 1
                        cnt = 1
                    op.tick = (epoch, cnt)
            block = es.enter_context(nc.Block())
            engmap = {"pe": block.tensor, "act": block.scalar, "dve": block.vector,
                      "pool": block.gpsimd, "sp": block.sync}

            def make(e):
                def body(eng):
                    waited = {}

                    def wait(sem, key, val):
                        if waited.get(key, 0) >= val:
                            return
                        eng.wait_ge(sem, val)
                        waited[key] = val

                    for op in self.ops[e]:
                        for d in op.deps:
                            if d.dma:
                                wait(dsems[d.dsem], ("d", d.dsem), d.dval)
                            else:
                                ep, t = d.tick
                                wait(esems[d.eng][ep], (d.eng, ep), t)
                        if op.dma:
                            if op.dval > 16:
                                wait(dsems[op.dsem], ("d", op.dsem), op.dval - 16)
                            ins = op.fn(eng)
                            ins.then_inc(dsems[op.dsem], 16)
                        elif op.fn is not None:
                            ins = op.fn(eng)
                            if op.needed:
                                ep, t = op.tick
                                ins.then_inc(esems[e][ep], 1)
                    for op in self.ops[e]:
                        if op.dma:
                            wait(dsems[op.dsem], ("d", op.dsem), op.dval)
                return body

            for e in ENGS:
                engmap[e](make(e))


class SbAlloc:
    def __init__(self, nc, start=16640, limit=229376 - 256):
        self.nc = nc
        self.limit = limit
        self.base = start
        self.cur = start
        self.n = 0

    def _al(self, name, shape, dt, persistent):
        nbytes = int(np.prod(shape[1:])) * (4 if dt == F32 else 2)
        nbytes = (nbytes + 63) // 64 * 64
        off = self.cur
        if off + nbytes > self.limit:
            raise RuntimeError(f"SBUF overflow allocating {name}: {off}+{nbytes}")
        self.cur += nbytes
        if persistent:
            assert self.base == off, "persistent allocs must come first"
            self.base = self.cur
        self.n += 1
        return self.nc.alloc_sbuf_tensor_at(f"{name}_{self.n}", list(shape), dt, offset=off)

    def pers(self, name, shape, dt):
        return self._al(name, shape, dt, True)

    def ph(self, name, shape, dt):
        return self._al(name, shape, dt, False)

    def reset(self):
        self.cur = self.base


def build(S, DEPTH):
    NT = S // 128
    NG = S // TG
    NHALF = 2 if NG >= 2 else 1
    GH = NG // NHALF
    HS = GH * TG

    nc = bass.Bass("TRN2", target_bir_lowering=False)
    dram_in = lambda name, shape: nc.dram_tensor(name, list(shape), F32, kind="ExternalInput").ap()
    x_d = dram_in("x", [S, D])
    c_d = dram_in("c_col", [128, KC])
    wada_d = dram_in("w_ada", [DEPTH, D, NMOD])
    bada_d = dram_in("b_ada_col", [DEPTH, 128, 48])
    nm_d = dram_in("nm_col", [DEPTH, 128, KC])
    nf_d = dram_in("nf_col", [DEPTH, 128, KC])
    win_d = dram_in("w_in", [DEPTH, D, DIN])
    w2_d = dram_in("w2aug", [DEPTH, 32, 256])
    qg_d = dram_in("qg_col", [128, DEPTH])
    kg_d = dram_in("kg_col", [128, DEPTH])
    gg_d = dram_in("gg_col", [128, DEPTH])
    wout_d = dram_in("w_out", [DEPTH, D, D])
    wr_d = dram_in("w_r", [DEPTH, D, 36])
    br_d = dram_in("b_rT", [36, DEPTH])
    wg_d = dram_in("w_gate", [DEPTH, 32, D, 256])
    wu_d = dram_in("w_up", [DEPTH, 32, D, 256])
    wd_d = dram_in("w_down", [DEPTH, 32, 256, D])
    out_d = nc.dram_tensor("out", [S, D], F32, kind="ExternalOutput").ap()

    xT_d = nc.dram_tensor("xT_scr", [D, S], F32).ap()
    qT_d = nc.dram_tensor("qT_scr", [512, S], BF16).ap()
    kT_d = nc.dram_tensor("kT_scr", [512, S], BF16).ap()
    v_d = nc.dram_tensor("v_scr", [S, 512], BF16).ap()
    mixT_d = nc.dram_tensor("mixT_scr", [D, S], BF16).ap()

    xT_v = xT_d.rearrange("(k p) s -> p k s", p=128)
    mixT_v = mixT_d.rearrange("(k p) s -> p k s", p=128)

    P = Prog(nc)
    A = SbAlloc(nc)
    banks = [nc.alloc_psum_tensor(f"bank{i}", [128, 512], F32) for i in range(8)]

    def MM(out, lhsT, rhs, start, stop, r, w):
        P.add("pe", lambda e: e.matmul(out, lhsT=lhsT, rhs=rhs, start=start, stop=stop), r, w)

    def TR(out, in_, ident, r, w):
        P.add("pe", lambda e: e.transpose(out=out, in_=in_, identity=ident), r, w)

    def ACT(out, in_, func, r, w, bias=0.0, scale=1.0, accum_out=None):
        if accum_out is None:
            P.add("act", lambda e: e.activation(out=out, in_=in_, func=func, bias=bias, scale=scale), r, w)
        else:
            P.add("act", lambda e: e.activation(out=out, in_=in_, func=func, bias=bias, scale=scale,
                                                accum_out=accum_out), r, w)

    def STT(out, in0, scalar, in1, op0, op1, r, w):
        P.add("dve", lambda e: e.scalar_tensor_tensor(out=out, in0=in0, scalar=scalar, in1=in1,
                                                      op0=op0, op1=op1), r, w)

    def TS(out, in0, s1, s2, op0, op1, r, w, eng="dve"):
        if s2 is None:
            P.add(eng, lambda e: e.tensor_scalar(out=out, in0=in0, scalar1=s1, scalar2=None, op0=op0), r, w)
        else:
            P.add(eng, lambda e: e.tensor_scalar(out=out, in0=in0, scalar1=s1, scalar2=s2, op0=op0, op1=op1), r, w)

    def TT(out, in0, in1, op, r, w, eng="dve"):
        P.add(eng, lambda e: e.tensor_tensor(out=out, in0=in0, in1=in1, op=op), r, w)

    def CP(out, in_, r, w, eng="dve"):
        P.add(eng, lambda e: e.tensor_copy(out=out, in_=in_), r, w)

    def RMAX(out, in_, r, w):
        P.add("dve", lambda e: e.tensor_reduce(out=out, in_=in_, axis=AX.X, op=ALU.max), r, w)

    def RECIP(out, in_, r, w):
        P.add("dve", lambda e: e.reciprocal(out=out, in_=in_), r, w)

    def MEMSET(t, val, w, eng="pool"):
        P.add(eng, lambda e: e.memset(t, val), (), w)

    def ASEL(out, in_, pattern, cmp, fill, base, cm, r, w):
        P.add("pool", lambda e: e.affine_select(out=out, in_=in_, pattern=pattern, compare_op=cmp,
                                                fill=fill, base=base, channel_multiplier=cm), r, w)

    def DMA(eng, out, in_, r, w):
        P.add(eng, lambda e: e.dma_start(out=out, in_=in_), r, w, dma=True)

    ident = A.pers("ident", [128, 128], F32)
    onesD = A.pers("onesD", [128, 128], BF16)
    blk64 = A.pers("blk64", [128, 128], BF16)
    onesV = A.pers("onesV", [128, 128], BF16)
    ntri = A.pers("ntri", [128, 128], BF16)
    nones = A.pers("nones", [128, 128], BF16)
    gtri = A.pers("gtri", [128, 128], BF16)
    gstr = A.pers("gstr", [128, 128], BF16)
    minc = A.pers("minc", [128, 128], BF16)
    maskS = A.pers("maskS", [128, 4, 512], BF16)
    sel = A.pers("sel", [32, 32, 128], BF16)
    one11 = A.pers("one11", [1, 1], F32)
    c_col = A.pers("c_col", [128, KC], F32)
    c_act = A.pers("c_act", [128, KC], F32)
    modT = A.pers("modT", [128, DEPTH, 48], F32)
    gsm = A.pers("gsm", [128, DEPTH, KC], F32)
    gsf = A.pers("gsf", [128, DEPTH, KC], F32)
    nmc = A.pers("nmc", [128, DEPTH, KC], F32)
    nfc = A.pers("nfc", [128, DEPTH, KC], F32)
    badac = A.pers("badac", [128, DEPTH, 48], F32)
    qgc = A.pers("qgc", [128, DEPTH], F32)
    kgc = A.pers("kgc", [128, DEPTH], F32)
    ggc = A.pers("ggc", [128, DEPTH], F32)
    w2a = A.pers("w2a", [32, DEPTH, 256], BF16)
    rbT = A.pers("rbT", [36, DEPTH], F32)

    MEMSET(ident[:], 0.0, ["ident"])
    ASEL(ident[:], ident[:], [[-1, 128]], ALU.not_equal, 1.0, 0, 1, ["ident"], ["ident"])
    MEMSET(onesD[:], 1.0 / 1024.0, ["onesD"])
    MEMSET(onesV[:], 1.0 / 128.0, ["onesV"])
    MEMSET(nones[:], -1.0, ["nones"])
    MEMSET(one11[:], 1.0, ["one11"])
    MEMSET(blk64[:], 0.0, ["blk64"])
    MEMSET(blk64[0:64, 0:64], 1.0 / 64.0, ["blk64"])
    MEMSET(blk64[64:128, 64:128], 1.0 / 64.0, ["blk64"])
    MEMSET(ntri[:], -1.0, ["ntri"])
    ASEL(ntri[:], ntri[:], [[-1, 128]], ALU.is_ge, 0.0, 0, 1, ["ntri"], ["ntri"])
    MEMSET(gtri[:], -1.0 / 16.0, ["gtri"])
    ASEL(gtri[:], gtri[:], [[1, 128]], ALU.is_ge, 0.0, 0, -1, ["gtri"], ["gtri"])
    MEMSET(gstr[:], -1.0 / 16.0, ["gstr"])
    ASEL(gstr[:], gstr[:], [[-1, 128]], ALU.is_gt, 0.0, 0, 1, ["gstr"], ["gstr"])
    MEMSET(minc[:], 1.0, ["minc"])
    ASEL(minc[:], minc[:], [[1, 128]], ALU.is_ge, 0.0, 0, -1, ["minc"], ["minc"])
    MEMSET(maskS[:], 1.0, ["maskS"])
    ASEL(maskS[:], maskS[:], [[-128, 4], [1, 512]], ALU.is_gt, 0.0, 0, -1, ["maskS"], ["maskS"])
    MEMSET(sel[:], 0.0, ["sel"])
    ASEL(sel[:], sel[:], [[-1, 32], [0, 128]], ALU.not_equal, 1.0, 0, 1, ["sel"], ["sel"])

    DMA("sp", c_col[:], c_d, [], ["c_col"])
    DMA("sp", nmc[:], nm_d.rearrange("l p k -> p l k"), [], ["nmc"])
    DMA("sp", nfc[:], nf_d.rearrange("l p k -> p l k"), [], ["nfc"])
    DMA("sp", badac[:], bada_d.rearrange("l p k -> p l k"), [], ["badac"])
    DMA("sp", qgc[:], qg_d, [], ["qgc"])
    DMA("sp", kgc[:], kg_d, [], ["kgc"])
    DMA("sp", ggc[:], gg_d, [], ["ggc"])
    DMA("pool", w2a[:], w2_d.rearrange("l p n -> p l n"), [], ["w2a"])
    DMA("sp", rbT[:], br_d, [], ["rbT"])
    ACT(c_act[:], c_col[:], AF.Silu, ["c_col"], ["c_act"])
    TS(qgc[:], qgc[:], 0.125, None, ALU.mult, None, ["qgc"], ["qgc"])

    A.reset()
    xin = [A.ph(f"xin{i}", [128, 4, D], F32) for i in range(2)]
    xst = [A.ph(f"xst{i}", [128, KC, TG], F32) for i in range(2)]
    x_v = x_d.rearrange("(t p) d -> p t d", p=128)
    for g in range(NG):
        s = g % 2
        DMA("sp", xin[s][:], x_v[:, g * 4:(g + 1) * 4, :], [], [f"xin{s}"])
        for k in range(KC):
            b = k % 2
            for t in range(4):
                TR(banks[b][:, t * 128:(t + 1) * 128], xin[s][:, t, k * 128:(k + 1) * 128], ident[:],
                   [f"xin{s}", "ident"], [f"pb{b}"])
            if k % 2 == 0:
                CP(xst[s][:, k, :], banks[b][:, :], [f"pb{b}"], [f"xst{s}"])
            else:
                ACT(xst[s][:, k, :], banks[b][:, :], AF.Identity, [f"pb{b}"], [f"xst{s}"])
        DMA("sp", xT_v[:, :, g * TG:(g + 1) * TG], xst[s][:], [f"xst{s}"], ["xT"])
    P.barrier()

    A.reset()
    wa = [A.ph(f"wa{i}", [128, KC, 512], F32) for i in range(2)]
    mrow = [A.ph(f"mrow{i}", [1, 512], F32) for i in range(2)]
    cnt = 0
    for l in range(1):
        wa_v = wada_d[l].rearrange("(k p) n -> p k n", p=128)
        for cg in range(12):
            s = cnt % 2
            cnt += 1
            DMA("sp", wa[s][:], wa_v[:, :, cg * 512:(cg + 1) * 512], [], [f"wa{s}"])
            for k in range(KC):
                MM(banks[s][0:1, :], c_act[:, k:k + 1], wa[s][:, k, :], k == 0, k == KC - 1,
                   ["c_act", f"wa{s}"], [f"pb{s}"])
            CP(mrow[s][:], banks[s][0:1, :], [f"pb{s}"], [f"mrow{s}"])
            for i in range(4):
                j = cg * 4 + i
                MM(banks[2 + l % 2][:, j:j + 1], mrow[s][0:1, i * 128:(i + 1) * 128], one11[0:1, 0:1], True, True,
                   [f"mrow{s}", "one11"], [f"pmod{l % 2}"])
        TT(modT[:, l, :], banks[2 + l % 2][:, 0:48], badac[:, l, :], ALU.add, [f"pmod{l % 2}", "badac"], ["modT"])
        STT(gsm[:, l, :], modT[:, l, 8:16], 1.0, nmc[:, l, :], ALU.add, ALU.mult, ["modT", "nmc"], ["gsm"])
        STT(gsf[:, l, :], modT[:, l, 32:40], 1.0, nfc[:, l, :], ALU.add, ALU.mult, ["modT", "nfc"], ["gsf"])
    P.barrier()

    def norm_group(xs, xs_key, gs_ap, shift_ap, ms_bank, ms_key, sq, lnv, rstd, tmp32, emit_k):
        for k in range(KC):
            q = k % 2
            ACT(sq[q][:], xs(k), AF.Square, [xs_key], [f"sq{q}"])
            MM(ms_bank[:, :], onesD[:], sq[q][:], k == 0, k == KC - 1, ["onesD", f"sq{q}"], [ms_key])
        ACT(lnv[:], ms_bank[:, :], AF.Ln, [ms_key], ["lnv"], bias=EPS)
        ACT(rstd[:], lnv[:], AF.Exp, ["lnv"], ["rstd"], scale=-0.5)
        for k in range(KC):
            q = k % 2
            STT(tmp32[q][:], xs(k), gs_ap[:, k:k + 1], rstd[:], ALU.mult, ALU.mult,
                [xs_key, "rstd", "gsm", "gsf"], [f"tmp32{q}"])
            emit_k(k, tmp32[q], f"tmp32{q}", shift_ap[:, k:k + 1])

    for l in range(DEPTH):
        A.reset()
        win = A.ph("win", [128, KC, DIN], BF16)
        xg = [A.ph(f"xg{i}", [128, KC, TG], F32) for i in range(2)]
        hT = [A.ph(f"hT{i}", [128, KC, TG], BF16) for i in range(2)]
        sq = [A.ph(f"sq{i}", [128, TG], BF16) for i in range(4)]
        lnv = A.ph("lnv", [128, TG], F32)
        rstd = A.ph("rstd", [128, TG], F32)
        tmp32 = [A.ph(f"tmp32{i}", [128, TG], F32) for i in range(2)]
        sqh = [A.ph(f"sqh{i}", [128, TG], BF16) for i in range(2)]
        lnh = [A.ph(f"lnh{i}", [128, TG], F32) for i in range(2)]
        rsh = [A.ph(f"rsh{i}", [128, TG], F32) for i in range(2)]
        qkst = [A.ph(f"qkst{i}", [128, TG], BF16) for i in range(2)]
        vst = [A.ph("vst0", [128, 4, 512], BF16)] * 2
        gq = [A.ph(f"gq{i}", [128, 2, TG], BF16) for i in range(2)]
        gk = [A.ph(f"gk{i}", [128, 2, TG], BF16) for i in range(2)]
        grs = [A.ph(f"grs{i}", [128, 4, TG], BF16) for i in range(2)]
        glr = [A.ph(f"glr{i}", [32, TG], BF16) for i in range(2)]
        gkt = [A.ph(f"gkt{i}", [128, 4, 256], BF16) for i in range(2)]
        gv = [A.ph(f"gv{i}", [128, 4, 512], BF16) for i in range(2)]
        ge = A.ph("ge", [128, 256], F32)
        lt = [A.ph(f"lt{i}", [128, 256], BF16) for i in range(2)]
        eb = [A.ph(f"eb{i}", [128, 128], F32) for i in range(2)]
        enb = [A.ph(f"enb{i}", [128, 128], F32) for i in range(2)]
        erem = [A.ph(f"erem{i}", [128, 128], F32) for i in range(2)]
        qd = [A.ph(f"qd{i}", [128, 128], BF16) for i in range(2)]
        ki = [A.ph(f"ki{i}", [128, 128], BF16) for i in range(2)]
        kd = [A.ph(f"kd{i}", [128, 128], BF16) for i in range(2)]
        scm = [A.ph(f"scm{i}", [128, 128], BF16) for i in range(4)]
        state = [A.ph(f"state{i}", [128, 256], F32) for i in range(2)]
        stbf = [A.ph(f"stbf{i}", [128, 256], BF16) for i in range(2)]
        osq = A.ph("osq", [128, 512], BF16)
        oln = A.ph("oln", [128, 512], F32)
        ors = A.ph("ors", [128, 512], F32)
        on32 = A.ph("on32", [128, 512], F32)
        obg = [A.ph(f"obg{i}", [128, 4, TG], BF16) for i in range(2)]

        win_v = win_d[l].rearrange("(k p) n -> p k n", p=128)
        for c0_, c1_ in ((0, 512), (1024, 1536), (1536, 2048), (512, 1024), (2048, 2560), (2560, DIN)):
            DMA("pool", win[:, :, c0_:c1_], win_v[:, :, c0_:c1_], [], [f"win{c0_ // 512}"])
        for i in range(2):
            MEMSET(glr[i][:], 1.0, [f"glr{i}"])
        for hp in range(2):
            MEMSET(state[hp][:], 0.0, [f"state{hp}"])
            MEMSET(stbf[hp][:], 0.0, [f"stbf{hp}"])
        v_v4 = v_d.rearrange("(t p) c -> p t c", p=128)

        def pump(gen, n):
            if gen is None:
                return None
            for _ in range(n):
                try:
                    next(gen)
                except StopIteration:
                    return None
            return gen

        def gen_norm(g):
            s = g % 2
            gs_ = slice(g * TG, (g + 1) * TG)
            for k in range(4):
                ACT(sq[k][:], xg[s][:, k, :], AF.Square, [f"xg{s}"], [f"sq{k}"])
                if k % 2 == 1:
                    yield
            for k in range(4):
                MM(banks[3][:, :], onesD[:], sq[k][:], k == 0, False, ["onesD", f"sq{k}"], ["pb3"])
            for k in range(4, KC):
                ACT(sq[k - 4][:], xg[s][:, k, :], AF.Square, [f"xg{s}"], [f"sq{k - 4}"])
            for k in range(4, KC):
                MM(banks[3][:, :], onesD[:], sq[k - 4][:], False, k == KC - 1, ["onesD", f"sq{k - 4}"], ["pb3"])
            ACT(lnv[:], banks[3][:, :], AF.Ln, ["pb3"], ["lnv"], bias=EPS)
            ACT(rstd[:], lnv[:], AF.Exp, ["lnv"], ["rstd"], scale=-0.5)
            yield
            for k in range(KC):
                q = k % 2
                STT(tmp32[q][:], xg[s][:, k, :], gsm[:, l, k:k + 1], rstd[:], ALU.mult, ALU.mult,
                    [f"xg{s}", "rstd", "gsm"], [f"tmp32{q}"])
                ACT(hT[s][:, k, :], tmp32[q][:], AF.Identity, [f"tmp32{q}", "modT"], [f"hT{s}"],
                    bias=modT[:, l, k:k + 1])
                yield

        def gen_proj(g):
            s = g % 2
            gs_ = slice(g * TG, (g + 1) * TG)

            def fm_unit(kind, i, c0, b, q_):
                pb = banks[b]
                M = 16 if kind == "glr" else 128
                for k in range(KC):
                    MM(pb[0:M, :], win[:, k, c0:c0 + M], hT[s][:, k, :], k == 0, k == KC - 1,
                       [f"win{min(c0 // 512, 5)}", f"hT{s}"], [f"pb{b}"])
                if kind in ("q", "k"):
                    gain = qgc[:, l:l + 1] if kind == "q" else kgc[:, l:l + 1]
                    ACT(sqh[q_][:], pb[:, :], AF.Square, [f"pb{b}"], [f"sqh{q_}"])
                    MM(banks[3][:, :], blk64[:], sqh[q_][:], True, True, ["blk64", f"sqh{q_}"], ["pb3"])
                    ACT(lnh[q_][:], banks[3][:, :], AF.Ln, ["pb3"], [f"lnh{q_}"], bias=EPS)
                    ACT(rsh[q_][:], lnh[q_][:], AF.Exp, [f"lnh{q_}"], [f"rsh{q_}"], scale=-0.5)
                    STT(qkst[q_][:], pb[:, :], gain, rsh[q_][:], ALU.mult, ALU.mult,
                        [f"pb{b}", f"rsh{q_}", "qgc", "kgc"], [f"qkst{q_}"])
                    dst = qT_d if kind == "q" else kT_d
                    DMA("sp", dst[i * 128:(i + 1) * 128, gs_], qkst[q_][:], [f"qkst{q_}"], [f"{kind}T"])
                elif kind == "gq":
                    CP(gq[s][:, i, :], pb[:, :], [f"pb{b}"], [f"gq{s}"])
                elif kind == "gk":
                    CP(gk[s][:, i, :], pb[:, :], [f"pb{b}"], [f"gk{s}"])
                elif kind == "gr":
                    ACT(grs[s][:, i, :], pb[:, :], AF.Silu, [f"pb{b}"], [f"grs{s}"])
                else:
                    CP(glr[s][0:16, :], pb[0:16, :], [f"pb{b}"], [f"glr{s}"])

            def tm_unit(t, j, b):
                ts_ = slice(t * 128, (t + 1) * 128)
                c0, n, dstt, dkey = ((1024, 512, vst, "vst"), (1792, 256, gkt, "gkt"), (2048, 512, gv, "gv"))[j]
                for k in range(KC):
                    MM(banks[b][:, 0:n], hT[s][:, k, ts_], win[:, k, c0:c0 + n], k == 0, k == KC - 1,
                       [f"win{min(c0 // 512, 5)}", f"hT{s}"], [f"pb{b}"])
                okey = "vst0" if dkey == "vst" else f"{dkey}{s}"
                if j == 1:
                    CP(dstt[s][:, t, :], banks[b][:, 0:n], [f"pb{b}"], [okey])
                else:
                    ACT(dstt[s][:, t, :], banks[b][:, 0:n], AF.Identity, [f"pb{b}"], [okey])

            qk_units = [("q", i, i * 128) for i in range(4)] + [("k", i, 512 + i * 128) for i in range(4)]
            oth_units = ([("gq", i, 1536 + i * 128) for i in range(2)] + [("gk", i, 1792 + i * 128) for i in range(2)]
                         + [("gr", i, 2560 + i * 128) for i in range(4)] + [("glr", 0, 3072)])
            tm_units = [(t, j) for t in range(4) for j in range(3)]
            ti = 0
            for n_ in range(9):
                if n_ < 8:
                    kind, i, c0 = qk_units[n_]
                    fm_unit(kind, i, c0, 1, n_ % 2)
                    yield
                if ti < len(tm_units):
                    tm_unit(tm_units[ti][0], tm_units[ti][1], 4 if ti % 2 == 0 else 0)
                    ti += 1
                    yield
                kind, i, c0 = oth_units[n_]
                fm_unit(kind, i, c0, 2, 0)
                yield
            while ti < len(tm_units):
                tm_unit(tm_units[ti][0], tm_units[ti][1], 4 if ti % 2 == 0 else 0)
                ti += 1
                yield
            DMA("sp", v_v4[:, g * 4:(g + 1) * 4, :], vst[0][:], ["vst0"], ["v"])

        def gen_gla(g):
            s = g % 2
            gs_ = slice(g * TG, (g + 1) * TG)
            for t in range(4):
                ts_ = slice(t * 128, (t + 1) * 128)
                lq = t % 2
                MM(banks[3][:, 0:256], glr[s][0:32, ts_], w2a[0:32, l, :], True, True, [f"glr{s}", "w2a"], ["pb3"])
                ACT(ge[:], banks[3][:, 0:256], AF.Exp, ["pb3"], ["ge"], scale=-1.0)
                ACT(lt[lq][:], ge[:], AF.Ln, ["ge"], [f"lt{lq}"], bias=1.0)
                yield
                def chain(hp, bank, bkey):
                    hs_ = slice(hp * 128, (hp + 1) * 128)
                    MM(bank[:, 0:128], lt[lq][:, hs_], gtri[:], True, True, [f"lt{lq}", "gtri"], [bkey])
                    MM(bank[:, 128:256], gstr[:], lt[lq][:, hs_], True, True, [f"lt{lq}", "gstr"], [bkey])
                    ACT(eb[hp][:], bank[:, 0:128], AF.Exp, [bkey], [f"eb{hp}"])
                    ACT(enb[hp][:], bank[:, 0:128], AF.Exp, [bkey], [f"enb{hp}"], scale=-1.0)
                    ACT(erem[hp][:], bank[:, 128:256], AF.Exp, [bkey], [f"erem{hp}"])
                    yield
                    STT(qd[hp][:], eb[hp][:], 0.125, gq[s][:, hp, ts_], ALU.mult, ALU.mult,
                        [f"eb{hp}", f"gq{s}"], [f"qd{hp}"])
                    TT(ki[hp][:], enb[hp][:], gk[s][:, hp, ts_], ALU.mult, [f"enb{hp}", f"gk{s}"], [f"ki{hp}"])
                    TT(kd[hp][:], erem[hp][:], gkt[s][:, t, hs_], ALU.mult, [f"erem{hp}", f"gkt{s}"], [f"kd{hp}"])
                    yield
                    for a in range(2):
                        h = 2 * hp + a
                        pa = slice(a * 64, (a + 1) * 64)
                        scb = bank[:, 256 + a * 128:256 + (a + 1) * 128]
                        MM(scb, ki[hp][pa, :], qd[hp][pa, :], True, True, [f"ki{hp}", f"qd{hp}"], [bkey])
                        TT(scm[h][:], scb, minc[:], ALU.mult, [bkey, "minc"], [f"scm{h}"])
                        yield
                        ob = banks[7][:, h * 128:(h + 1) * 128]
                        MM(ob, gv[s][:, t, h * 128:(h + 1) * 128], scm[h][:], True, False,
                           [f"gv{s}", f"scm{h}"], ["pb7"])
                        MM(ob, stbf[hp][pa, a * 128:(a + 1) * 128], qd[hp][pa, :], False, True,
                           [f"stbf{hp}", f"qd{hp}"], ["pb7"])
                    MM(bank[:, 0:256], kd[hp][:], gv[s][:, t, hp * 256:(hp + 1) * 256], True, True,
                       [f"kd{hp}", f"gv{s}"], [bkey])
                    STT(state[hp][:], state[hp][:], eb[hp][:, 127:128], bank[:, 0:256], ALU.mult, ALU.add,
                        [f"state{hp}", f"eb{hp}", bkey], [f"state{hp}"])
                    ACT(stbf[hp][:], state[hp][:], AF.Identity, [f"state{hp}"], [f"stbf{hp}"])
                    yield

                c0_, c1_ = chain(0, banks[6], "pb6"), chain(1, banks[5], "pb5")
                while c0_ is not None or c1_ is not None:
                    c0_ = pump(c0_, 1)
                    c1_ = pump(c1_, 1)
                    yield
                ACT(osq[:], banks[7][:, :], AF.Square, ["pb7"], ["osq"])
                MM(banks[3][:, :], onesV[:], osq[:], True, True, ["onesV", "osq"], ["pb3"])
                ACT(oln[:], banks[3][:, :], AF.Ln, ["pb3"], ["oln"], bias=EPS)
                ACT(ors[:], oln[:], AF.Exp, ["oln"], ["ors"], scale=-0.5)
                yield
                STT(on32[:], banks[7][:, :], ggc[:, l:l + 1], ors[:], ALU.mult, ALU.mult,
                    ["pb7", "ors", "ggc"], ["on32"])
                for h in range(4):
                    TT(obg[s][:, h, ts_], on32[:, h * 128:(h + 1) * 128], grs[s][:, h, ts_], ALU.mult,
                       ["on32", f"grs{s}"], [f"obg{s}"], eng="pool")
                yield
            DMA("sp", mixT_v[:, 4:8, gs_], obg[s][:], [f"obg{s}"], ["mixT"])

        def load_xg(g):
            DMA("sp", xg[g % 2][:], xT_v[:, :, g * TG:(g + 1) * TG], ["xT"], [f"xg{g % 2}"])

        load_xg(0)
        if NG > 1:
            load_xg(1)
        for step in range(-1, NG + 1):
            if step >= 0 and step + 2 < NG:
                load_xg(step + 2)
            gn = gen_norm(step + 1) if 0 <= step + 1 < NG else None
            gp = gen_proj(step) if 0 <= step < NG else None
            gl_ = gen_gla(step - 1) if 0 <= step - 1 < NG else None
            cyc = 0
            while gn is not None or gp is not None or gl_ is not None:
                gp = pump(gp, 1)
                gl_ = pump(gl_, 1)
                cyc += 1
                if cyc % 4 == 0 or gp is None:
                    gl_ = pump(gl_, 1)
                if cyc % 2 == 0 or gp is None:
                    gn = pump(gn, 1)
        P.barrier()

        A.reset()
        WOUT_OFF = 204480
        wout = nc.alloc_sbuf_tensor_at(f"wout_l{l}", [128, KC, D], BF16, offset=WOUT_OFF)
        wr = nc.alloc_sbuf_tensor_at(f"wr_l{l}", [128, KC, 36], F32, offset=WOUT_OFF + 16384)
        DMA("pool", wout[:], wout_d[l].rearrange("(k p) n -> p k n", p=128), [], ["wout"])
        DMA("sp", wr[:], wr_d[l].rearrange("(k p) n -> p k n", p=128), [], ["wr"])
        QT = [A.ph(f"QT{i}", [128, S], BF16) for i in range(2)]
        KT = [A.ph(f"KT{i}", [128, S], BF16) for i in range(2)]
        Vp = [A.ph(f"Vp{i}", [128, NT, 2, 128], BF16) for i in range(2)]
        ez = [A.ph(f"ez{i}", [128, TG], F32) for i in range(2)]
        spb = [A.ph(f"spb{i}", [128, TG], BF16) for i in range(4)]
        wb = [A.ph(f"wb{i}", [128, TG], BF16) for i in range(4)]
        lacc = [A.ph(f"lacc{i}", [128, TG], BF16) for i in range(2)]
        oast = [A.ph(f"oast{i}", [128, TG], BF16) for i in range(2)]
        for i in range(2):
            MEMSET(Vp[i][:], 0.0, [f"Vp{i}"])
        v_v = v_d.rearrange("(t p) c -> p t c", p=128)
        L2 = l + 1
        do_mod = L2 < DEPTH
        modst = {"next": 0, "steps": 0}
        if do_mod:
            wa2 = [A.ph(f"wa2_{i}", [128, KC, 256], F32) for i in range(2)]
            mrow2 = [A.ph(f"mrow2_{i}", [1, 256], F32) for i in range(2)]
            wa_v2 = wada_d[L2].rearrange("(k p) n -> p k n", p=128)
            MOD_EVERY = max(1, (4 * 2 * sum(4 * g_ + 4 for g_ in range(NG))) // 26)

            def mod_load(cg):
                DMA("sp", wa2[cg % 2][:], wa_v2[:, :, cg * 256:(cg + 1) * 256], [], [f"wa2_{cg % 2}"])

            def mod_chunk(cg):
                sl = cg % 2
                for k in range(KC):
                    MM(banks[7][0:1, 256:512], c_act[:, k:k + 1], wa2[sl][:, k, :], k == 0, k == KC - 1,
                       ["c_act", f"wa2_{sl}"], ["pb7"])
                CP(mrow2[sl][:], banks[7][0:1, 256:512], ["pb7"], [f"mrow2_{sl}"])
                for i in range(2):
                    j = cg * 2 + i
                    MM(banks[7][:, j:j + 1], mrow2[sl][0:1, i * 128:(i + 1) * 128], one11[0:1, 0:1], True, True,
                       [f"mrow2_{sl}", "one11"], ["pb7"])
                if cg + 2 < 24:
                    mod_load(cg + 2)

            def mod_finish():
                while modst["next"] < 24:
                    mod_chunk(modst["next"])
                    modst["next"] += 1
                TT(modT[:, L2, :], banks[7][:, 0:48], badac[:, L2, :], ALU.add, ["pb7", "badac"], ["modT"])
                STT(gsm[:, L2, :], modT[:, L2, 8:16], 1.0, nmc[:, L2, :], ALU.add, ALU.mult, ["modT", "nmc"], ["gsm"])
                STT(gsf[:, L2, :], modT[:, L2, 32:40], 1.0, nfc[:, L2, :], ALU.add, ALU.mult, ["modT", "nfc"], ["gsf"])

            mod_load(0)
            mod_load(1)
        def load_pair(hp_):
            s_ = hp_ % 2
            DMA("sp", QT[s_][:], qT_d[hp_ * 128:(hp_ + 1) * 128, :], ["qT"], [f"QT{s_}"])
            DMA("sp", KT[s_][:], kT_d[hp_ * 128:(hp_ + 1) * 128, :], ["kT"], [f"KT{s_}"])
            for a in range(2):
                h = 2 * hp_ + a
                for t0 in range(0, NT, 8):
                    t1 = min(NT, t0 + 8)
                    DMA("sp", Vp[s_][:, t0:t1, a, a * 64:(a + 1) * 64], v_v[:, t0:t1, h * 64:(h + 1) * 64],
                        ["v"], [f"Vp{s_}"])

        load_pair(0)
        for hp in range(4):
            s = hp % 2
            if hp + 1 < 4:
                load_pair(hp + 1)
            steps = []
            for g in range(NG):
                for kt in range(4 * g + 3, -1, -1):
                    for a in range(2):
                        steps.append((g, kt, a))
            NS = len(steps)

            def info(i):
                g, kt, a = steps[i]
                r = kt - 4 * g
                c0 = max(r, 0) * 128
                return g, kt, a, r, c0

            def st_z(i):
                g, kt, a, r, c0 = info(i)
                zq = i % 2
                pa = slice(a * 64, (a + 1) * 64)
                MM(banks[zq][:, c0:TG], KT[s][pa, kt * 128:(kt + 1) * 128], QT[s][pa, g * TG + c0:(g + 1) * TG],
                   True, True, [f"KT{s}", f"QT{s}"], [f"pb{zq}"])

            def st_ez(i):
                g, kt, a, r, c0 = info(i)
                zq = i % 2
                ACT(ez[zq][:, c0:TG], banks[zq][:, c0:TG], AF.Exp, [f"pb{zq}"], [f"ez{zq}"])

            def st_ln(i):
                g, kt, a, r, c0 = info(i)
                zq = i % 2
                s4 = i % 4
                ACT(spb[s4][:, c0:TG], ez[zq][:, c0:TG], AF.Ln, [f"ez{zq}"], [f"spb{s4}"], bias=1.0)
                if r >= 0:
                    TT(spb[s4][:, c0:TG], spb[s4][:, c0:TG], maskS[:, r, c0:TG], ALU.mult,
                       [f"spb{s4}", "maskS"], [f"spb{s4}"])

            def st_lw(i):
                g, kt, a, r, c0 = info(i)
                s4 = i % 4
                lq = 2 + i % 3
                pa = slice(a * 64, (a + 1) * 64)
                fog = (kt == 4 * g + 3)
                lb = banks[lq]
                MM(lb[:, c0:TG], KT[s][pa, kt * 128:(kt + 1) * 128], QT[s][pa, g * TG + c0:(g + 1) * TG],
                   True, False, [f"KT{s}", f"QT{s}"], [f"pb{lq}"])
                MM(lb[:, c0:TG], ntri[:], spb[s4][:, c0:TG], False, fog, ["ntri", f"spb{s4}"], [f"pb{lq}"])
                if not fog:
                    MM(lb[:, c0:TG], nones[:], lacc[a][:, c0:TG], False, True, ["nones", f"lacc{a}"], [f"pb{lq}"])
                if kt > 0:
                    if fog:
                        if c0 > 0:
                            MEMSET(lacc[a][:, 0:c0], 0.0, [f"lacc{a}"])
                        CP(lacc[a][:, c0:TG], spb[s4][:, c0:TG], [f"spb{s4}"], [f"lacc{a}"])
                    else:
                        TT(lacc[a][:, c0:TG], lacc[a][:, c0:TG], spb[s4][:, c0:TG], ALU.add,
                           [f"lacc{a}", f"spb{s4}"], [f"lacc{a}"])

            def st_ew(i):
                g, kt, a, r, c0 = info(i)
                lq = 2 + i % 3
                w4 = i % 4
                ACT(wb[w4][:, c0:TG], banks[lq][:, c0:TG], AF.Exp, [f"pb{lq}"], [f"wb{w4}"])
                if r >= 0:
                    TT(wb[w4][:, c0:TG], wb[w4][:, c0:TG], maskS[:, r, c0:TG], ALU.mult,
                       [f"wb{w4}", "maskS"], [f"wb{w4}"])

            def st_acc(i):
                g, kt, a, r, c0 = info(i)
                w4 = i % 4
                ab = 5 + g % 2
                first = (kt == 4 * g + 3 and a == 0)
                last = (kt == 0 and a == 1)
                out_ap, lhsT_ap, rhs_ap = banks[ab][:, c0:TG], Vp[s][:, kt, a, :], wb[w4][:, c0:TG]
                P.add("pe", lambda e: e.matmul(out_ap, lhsT=lhsT_ap, rhs=rhs_ap, start=first, stop=last,
                                               skip_group_check=True),
                      [f"Vp{s}", f"wb{w4}"], [f"pb{ab}"])
                if last:
                    o = g % 2
                    CP(oast[o][:], banks[ab][:, :], [f"pb{ab}"], [f"oast{o}"])
                    DMA("sp", mixT_v[:, hp, g * TG:(g + 1) * TG], oast[o][:], [f"oast{o}"], ["mixT"])

            for i in range(-2, NS + 2):
                if do_mod:
                    modst["steps"] += 1
                    if modst["steps"] % MOD_EVERY == 0 and modst["next"] < 24:
                        mod_chunk(modst["next"])
                        modst["next"] += 1
                if 0 <= i + 2 < NS:
                    st_z(i + 2)
                if 0 <= i + 1 < NS:
                    st_ez(i + 1)
                if 0 <= i < NS:
                    st_lw(i)
                if 0 <= i - 1 < NS:
                    st_ew(i - 1)
                if 0 <= i + 1 < NS:
                    st_ln(i + 1)
                if 0 <= i - 2 < NS:
                    st_acc(i - 2)
        if do_mod:
            mod_finish()
        P.barrier()

        A.reset()
        x1 = A.ph("x1", [128, KC, HS], F32)
        h2T = A.ph("h2T", [128, KC, HS], BF16)
        combT = A.ph("combT", [32, HS], BF16)
        NW = 3
        wgb = [A.ph(f"wgb{i}", [128, KC, 256], BF16) for i in range(NW)]
        wub = [A.ph(f"wub{i}", [128, KC, 256], BF16) for i in range(NW)]
        wdb = [A.ph(f"wdb{i}", [128, 2, D], BF16) for i in range(NW)]
        mark = A.cur
        mix = [A.ph(f"mix{i}", [128, KC, TG], BF16) for i in range(1)]
        h32 = [A.ph(f"h32{i}", [128, TG], F32) for i in range(2)]
        lgT = A.ph("lgT", [36, TG], F32)
        sq = [A.ph(f"sq{i}", [128, TG], BF16) for i in range(2)]
        lnv = A.ph("lnv", [128, TG], F32)
        rstd = A.ph("rstd", [128, TG], F32)
        tmp32 = [A.ph(f"tmp32{i}", [128, TG], F32) for i in range(2)]
        top_d = A.cur
        A.cur = mark
        bcs = [A.ph(f"bcs{i}", [128, TG], F32) for i in range(2)]
        sil = [A.ph(f"sil{i}", [128, TG], F32) for i in range(2)]
        su = [A.ph(f"su{i}", [128, TG], F32) for i in range(2)]
        actb = [A.ph(f"actb{i}", [128, TG], BF16) for i in range(8)]
        A.cur = max(A.cur, top_d)
        rt4 = {n: A.ph(f"rt_{n}", [128, 4, w_], F32) for n, w_ in
              (("lg", 36), ("gmax", 1), ("ngmax", 1), ("ohg", 4), ("gex", 4), ("gsum", 1), ("grw", 1), ("pen", 4),
               ("elm", 32), ("m1", 1), ("oh1", 32), ("elm2", 32), ("m2", 1), ("oh2", 32), ("dd", 1), ("e2", 1),
               ("w1", 1), ("w1g", 1), ("w2g", 1), ("c1", 32), ("comb", 32))}
        rts = [{n: v[:, t, :] for n, v in rt4.items()} for t in range(4)]

        assert A.cur <= WOUT_OFF, A.cur

        def gen_op(half, gg):
            g = half * GH + gg
            gs_ = slice(g * TG, (g + 1) * TG)
            ls_ = slice(gg * TG, (gg + 1) * TG)
            xk = f"x1_{gg}"
            DMA("sp", mix[0][:], mixT_v[:, :, gs_], ["mixT"], ["mix0"])
            for oc in range(KC):
                b = oc % 2
                for k in range(KC):
                    MM(banks[b][:, :], wout[:, k, oc * 128:(oc + 1) * 128], mix[0][:, k, :], k == 0, k == KC - 1,
                       ["wout", "mix0"], [f"pb{b}"])
                STT(x1[:, oc, ls_], banks[b][:, :], modT[:, l, 16 + oc:17 + oc], x1[:, oc, ls_], ALU.mult, ALU.add,
                    [f"pb{b}", xk, "modT"], [xk])
                yield

        def gen_n2(half, gg):
            ls_ = slice(gg * TG, (gg + 1) * TG)
            xk = f"x1_{gg}"
            for k in range(KC):
                q = k % 2
                ACT(sq[q][:], x1[:, k, ls_], AF.Square, [xk], [f"sq{q}"])
                MM(banks[2][:, :], onesD[:], sq[q][:], k == 0, k == KC - 1, ["onesD", f"sq{q}"], ["pb2"])
                yield
            ACT(lnv[:], banks[2][:, :], AF.Ln, ["pb2"], ["lnv"], bias=EPS)
            ACT(rstd[:], lnv[:], AF.Exp, ["lnv"], ["rstd"], scale=-0.5)
            yield
            for k in range(KC):
                q = k % 2
                STT(tmp32[q][:], x1[:, k, ls_], gsf[:, l, k:k + 1], rstd[:], ALU.mult, ALU.mult,
                    [xk, "rstd", "gsf"], [f"tmp32{q}"])
                ACT(h32[q][:], tmp32[q][:], AF.Identity, [f"tmp32{q}", "modT"], [f"h32{q}"], bias=modT[:, l, 24 + k:25 + k])
                MM(banks[3][0:36, :], wr[:, k, :], h32[q][:], k == 0, k == KC - 1, ["wr", f"h32{q}"], ["pb3"])
                CP(h2T[:, k, ls_], h32[q][:], [f"h32{q}"], [f"h2T_{gg}"], eng="pool")
                yield
            ACT(lgT[:], banks[3][0:36, :], AF.Identity, ["pb3", "rbT"], ["lgT"], bias=rbT[0:36, l:l + 1])

        def rt_chain(t, ts_):
            R = rts[t]
            K_ = lambda n: f"{n}{t}"
            CP(R["lg"], banks[4][:, t * 36:(t + 1) * 36], ["pb4"], [K_("lg")])
            yield
            RMAX(R["gmax"], R["lg"][:, 0:4], [K_("lg")], [K_("gmax")])
            yield
            TS(R["ohg"], R["lg"][:, 0:4], R["gmax"][:, 0:1], None, ALU.is_equal, None, [K_("lg"), K_("gmax")], [K_("ohg")])
            TS(R["ngmax"], R["gmax"], -1.0, None, ALU.mult, None, [K_("gmax")], [K_("ngmax")])
            yield
            ACT(R["gex"], R["lg"][:, 0:4], AF.Exp, [K_("lg"), K_("ngmax")], [K_("gex"), K_("gsum")],
                bias=R["ngmax"][:, 0:1], accum_out=R["gsum"][:, 0:1])
            TS(R["pen"], R["ohg"], BIG, -BIG, ALU.mult, ALU.add, [K_("ohg")], [K_("pen")])
            yield
            RECIP(R["grw"], R["gsum"], [K_("gsum")], [K_("grw")])
            for gi in range(4):
                TS(R["elm"][:, gi * 8:(gi + 1) * 8], R["lg"][:, 4 + gi * 8:12 + gi * 8], R["pen"][:, gi:gi + 1],
                   None, ALU.add, None, [K_("lg"), K_("pen")], [K_("elm")])
            yield
            RMAX(R["m1"], R["elm"], [K_("elm")], [K_("m1")])
            yield
            TS(R["oh1"], R["elm"], R["m1"][:, 0:1], None, ALU.is_equal, None, [K_("elm"), K_("m1")], [K_("oh1")])
            yield
            STT(R["elm2"], R["oh1"], -BIG, R["elm"], ALU.mult, ALU.add, [K_("oh1"), K_("elm")], [K_("elm2")])
            yield
            RMAX(R["m2"], R["elm2"], [K_("elm2")], [K_("m2")])
            yield
            TS(R["oh2"], R["elm2"], R["m2"][:, 0:1], None, ALU.is_equal, None, [K_("elm2"), K_("m2")], [K_("oh2")])
            TT(R["dd"], R["m2"], R["m1"], ALU.subtract, [K_("m1"), K_("m2")], [K_("dd")])
            yield
            ACT(R["e2"], R["dd"], AF.Exp, [K_("dd")], [K_("e2")])
            yield
            TS(R["w1"], R["e2"], 1.0, None, ALU.add, None, [K_("e2")], [K_("w1")])
            yield
            RECIP(R["w1"], R["w1"], [K_("w1")], [K_("w1")])
            yield
            TT(R["w1g"], R["w1"], R["grw"], ALU.mult, [K_("w1"), K_("grw")], [K_("w1g")])
            yield
            TT(R["w2g"], R["e2"], R["w1g"], ALU.mult, [K_("e2"), K_("w1g")], [K_("w2g")])
            TS(R["c1"], R["oh1"], R["w1g"][:, 0:1], None, ALU.mult, None, [K_("oh1"), K_("w1g")], [K_("c1")])
            yield
            STT(R["comb"], R["oh2"], R["w2g"][:, 0:1], R["c1"], ALU.mult, ALU.add,
                [K_("oh2"), K_("w2g"), K_("c1")], [K_("comb")])
            yield
            TR(banks[5][0:32, ts_], R["comb"], ident[:], [K_("comb"), "ident"], ["pb5"])

        def gen_rt(half, gg):
            ls_ = slice(gg * TG, (gg + 1) * TG)
            for t in range(4):
                TR(banks[4][:, t * 36:(t + 1) * 36], lgT[0:36, t * 128:(t + 1) * 128], ident[0:36, 0:36],
                   ["lgT", "ident"], ["pb4"])
            yield
            chains = [rt_chain(t, slice(t * 128, (t + 1) * 128)) for t in range(4)]
            while any(c is not None for c in chains):
                chains = [pump2(c) for c in chains]
                yield
            CP(combT[0:32, ls_], banks[5][0:32, :], ["pb5"], ["combT"])

        def pump2(gen):
            if gen is None:
                return None
            try:
                next(gen)
            except StopIteration:
                return None
            return gen

        wstate = {"cnt": 0}

        def issue_weights(ep):
            slots = []
            for e_ in (2 * ep, 2 * ep + 1):
                ws = wstate["cnt"] % NW
                wstate["cnt"] += 1
                slots.append(ws)
                DMA("pool", wgb[ws][:], wg_d[l, e_].rearrange("(k p) f -> p k f", p=128), [], [f"wgb{ws}"])
                DMA("pool", wub[ws][:], wu_d[l, e_].rearrange("(k p) f -> p k f", p=128), [], [f"wub{ws}"])
                DMA("pool", wdb[ws][:], wd_d[l, e_].rearrange("(c p) n -> p c n", p=128), [], [f"wdb{ws}"])
            return slots

        for half in range(NHALF):
            slots0 = issue_weights(0)
            for gg_ in range(GH):
                g_ = half * GH + gg_
                DMA("sp", x1[:, :, gg_ * TG:(gg_ + 1) * TG], xT_v[:, :, g_ * TG:(g_ + 1) * TG], ["xT"], [f"x1_{gg_}"])
            for step in range(-1, GH + 1):
                go = gen_op(half, step + 1) if 0 <= step + 1 < GH else None
                gn2 = gen_n2(half, step) if 0 <= step < GH else None
                gr = gen_rt(half, step - 1) if 0 <= step - 1 < GH else None
                while go is not None or gn2 is not None or gr is not None:
                    go = pump2(go)
                    gn2 = pump2(gn2)
                    gn2 = pump2(gn2)
                    gr = pump2(gr)
                    gr = pump2(gr)
            P.barrier()
            def gate_up(ep, slots, gg, ab0):
                ls_ = slice(gg * TG, (gg + 1) * TG)
                for ei, e_ in enumerate((2 * ep, 2 * ep + 1)):
                    ws = slots[ei]
                    MM(banks[0][:, :], sel[0:32, e_, :], combT[0:32, ls_], True, True, ["sel", "combT"], ["pb0"])
                    ACT(bcs[ei][:], banks[0][:, :], AF.Identity, ["pb0"], [f"bcs{ei}"])
                    for fc in range(2):
                        fs_ = slice(fc * 128, (fc + 1) * 128)
                        q = fc
                        for k in range(KC):
                            MM(banks[1 + q][:, :], wgb[ws][:, k, fs_], h2T[:, k, ls_], k == 0, k == KC - 1,
                               [f"wgb{ws}", "h2T"], [f"pb{1 + q}"])
                        for k in range(KC):
                            MM(banks[3 + q][:, :], wub[ws][:, k, fs_], h2T[:, k, ls_], k == 0, k == KC - 1,
                               [f"wub{ws}", "h2T"], [f"pb{3 + q}"])
                        ACT(sil[q][:], banks[1 + q][:, :], AF.Silu, [f"pb{1 + q}"], [f"sil{q}"])
                        TT(su[q][:], sil[q][:], banks[3 + q][:, :], ALU.mult, [f"sil{q}", f"pb{3 + q}"], [f"su{q}"])
                        ai = ab0 + ei * 2 + fc
                        TT(actb[ai][:], su[q][:], bcs[ei][:], ALU.mult, [f"su{q}", f"bcs{ei}"], [f"actb{ai}"])

            def down(ep, slots, gg, ab0):
                ls_ = slice(gg * TG, (gg + 1) * TG)
                for oc in range(KC):
                    b = 5 + oc % 2
                    n = 0
                    for ei in range(2):
                        ws = slots[ei]
                        for fc in range(2):
                            ai = ab0 + ei * 2 + fc
                            MM(banks[b][:, :], wdb[ws][:, fc, oc * 128:(oc + 1) * 128], actb[ai][:], n == 0, n == 3,
                               [f"wdb{ws}", f"actb{ai}"], [f"pb{b}"])
                            n += 1
                    STT(x1[:, oc, ls_], banks[b][:, :], modT[:, l, 40 + oc:41 + oc], x1[:, oc, ls_],
                        ALU.mult, ALU.add, [f"pb{b}", f"x1_{gg}", "modT"], [f"x1_{gg}"])
                if ep == 15:
                    g_ = half * GH + gg
                    DMA("sp", xT_v[:, :, g_ * TG:(g_ + 1) * TG], x1[:, :, ls_], [f"x1_{gg}"], ["xT"])

            pending = None
            nset = 0
            for ep in range(16):
                slots = slots0 if ep == 0 else issue_weights(ep)
                for gg in range(GH):
                    ab0 = (nset % 2) * 4
                    nset += 1
                    gate_up(ep, slots, gg, ab0)
                    if pending is not None:
                        down(*pending)
                    pending = (ep, slots, gg, ab0)
                down(*pending)
                pending = None
            P.barrier()

    A.reset()
    xe = [A.ph(f"xe{i}", [128, KC, TG], F32) for i in range(2)]
    ost = [A.ph(f"ost{i}", [128, 4, D], F32) for i in range(2)]
    out_v = out_d.rearrange("(t p) d -> p t d", p=128)
    for g in range(NG):
        s = g % 2
        DMA("sp", xe[s][:], xT_v[:, :, g * TG:(g + 1) * TG], ["xT"], [f"xe{s}"])
        for t in range(4):
            for hk in range(2):
                b = (t * 2 + hk) % 2
                for kk in range(4):
                    k = hk * 4 + kk
                    TR(banks[b][:, kk * 128:(kk + 1) * 128], xe[s][:, k, t * 128:(t + 1) * 128], ident[:],
                       [f"xe{s}", "ident"], [f"pb{b}"])
                if hk == 0:
                    CP(ost[s][:, t, 0:512], banks[b][:, :], [f"pb{b}"], [f"ost{s}"])
                else:
                    ACT(ost[s][:, t, 512:1024], banks[b][:, :], AF.Identity, [f"pb{b}"], [f"ost{s}"])
        DMA("sp", out_v[:, g * 4:(g + 1) * 4, :], ost[s][:], [f"ost{s}"], ["out"])
    P.emit()
    return nc


def _col(v, n):
    return np.ascontiguousarray(np.asarray(v, dtype=np.float32).reshape(n, 128).T)


def make_in_maps(inputs, S, DEPTH, n_cores):
    f = lambda a: np.ascontiguousarray(np.asarray(a, dtype=np.float32))
    L = DEPTH
    shared = {
        "w_ada": f(inputs["w_ada"][:L]),
        "b_ada_col": np.stack([_col(inputs["b_ada"][l], 48) for l in range(L)]),
        "nm_col": np.stack([_col(inputs["norm_mix"][l], KC) for l in range(L)]),
        "nf_col": np.stack([_col(inputs["norm_ffn"][l], KC) for l in range(L)]),
        "w_in": f(inputs["w_in"][:L]),
        "w_out": f(inputs["w_out"][:L]),
        "w_gate": f(np.asarray(inputs["w_gate"][:L]).reshape(L, 32, D, 256)),
        "w_up": f(np.asarray(inputs["w_up"][:L]).reshape(L, 32, D, 256)),
        "w_down": f(np.asarray(inputs["w_down"][:L]).reshape(L, 32, 256, D)),
    }
    w2aug = np.zeros((L, 32, 256), np.float32)
    w2aug[:, 0:16, :] = np.asarray(inputs["w_gk2"][:L])
    w2aug[:, 16, :] = np.asarray(inputs["b_gk"][:L])
    shared["w2aug"] = w2aug
    rep2 = lambda v: np.concatenate([v, v]).astype(np.float32)
    shared["qg_col"] = f(np.stack([rep2(np.asarray(inputs["q_gain"][l])) for l in range(L)], axis=1))
    shared["kg_col"] = f(np.stack([rep2(np.asarray(inputs["k_gain"][l])) for l in range(L)], axis=1))
    shared["gg_col"] = f(np.stack([np.asarray(inputs["gla_gain"][l], np.float32) for l in range(L)], axis=1))
    shared["w_r"] = f(np.concatenate([np.asarray(inputs["w_router_grp"][:L]), np.asarray(inputs["w_router_exp"][:L])], axis=2))
    b_r = np.concatenate([np.asarray(inputs["b_router_grp"][:L]), np.asarray(inputs["b_router_exp"][:L])], axis=1)
    shared["b_rT"] = f(b_r.T)
    maps = []
    x = np.asarray(inputs["x"], dtype=np.float32)
    c = np.asarray(inputs["c"], dtype=np.float32)
    for b in range(n_cores):
        m = dict(shared)
        m["x"] = np.ascontiguousarray(x[b, :S])
        m["c_col"] = _col(c[b], KC)
        maps.append(m)
    return maps


_NC_CACHE = {}


def kernel(**inputs):
    x = np.asarray(inputs["x"])
    B, S, _ = x.shape
    DEPTH = np.asarray(inputs["w_ada"]).shape[0]
    key = (S, DEPTH)
    if key not in _NC_CACHE:
        _NC_CACHE[key] = build(S, DEPTH)
    nc = _NC_CACHE[key]
    maps = make_in_maps(inputs, S, DEPTH, B)
    res = run_bass_kernel_spmd(nc, maps, core_ids=list(range(B)))
    return np.stack([np.asarray(r["out"], dtype=np.float32) for r in res.results], axis=0)
```
